# Optimizing a Trainium2 kernel written in Bass

```python
import math
import jax, jax.numpy as jnp
from jax import lax
import numpy as np


D_MODEL = 2048
BATCH = 8
SEQ = 2048
DEPTH = 1

HEAD_DIM = 128
DIL_CONFIGS = ((128, 1), (512, 4), (2048, 16))
N_DIL_GROUPS = 3
HEADS_PER_DIL_GROUP = 4
HEADS_A = N_DIL_GROUPS * HEADS_PER_DIL_GROUP
N_KEYS_A = 129
HEADS_B = 8
N_ATTN_HEADS = HEADS_A + HEADS_B
IDX_HEADS = 16
IDX_DIM = 64
TOPK_MAX = 256
N_BUCKETS = 32
MAX_DISTANCE = 2048
D_FF = 4 * D_MODEL
WIDTH_A = HEADS_A * HEAD_DIM
WIDTH_B = HEADS_B * HEAD_DIM
WIDTH_A_OUT = HEADS_PER_DIL_GROUP * HEAD_DIM
D_IN = 3 * WIDTH_A + 3 * WIDTH_B + IDX_HEADS * IDX_DIM + IDX_DIM + IDX_HEADS + 2 * D_MODEL
Q_BLOCK = 64
NORM_EPS = 1e-6
NEG_INF = -1e30

kernel_name = "gated_dilated_dsa_hybrid_block"


def rms_norm(x, g):
    xf = x.astype(jnp.float32)
    y = xf * lax.rsqrt(jnp.mean(xf * xf, axis=-1, keepdims=True) + NORM_EPS) * g.astype(jnp.float32)
    return y.astype(x.dtype)


def layer_norm(x, g, b):
    xf = x.astype(jnp.float32)
    mu = jnp.mean(xf, axis=-1, keepdims=True)
    var = jnp.mean(jnp.square(xf - mu), axis=-1, keepdims=True)
    y = (xf - mu) * lax.rsqrt(var + NORM_EPS) * g.astype(jnp.float32) + b.astype(jnp.float32)
    return y.astype(x.dtype)


def t5_bucket(dist):
    n = jnp.maximum(dist, 0)
    max_exact = N_BUCKETS // 2
    nf = jnp.maximum(n, 1).astype(jnp.float32)
    large = max_exact + (jnp.log(nf / max_exact) / math.log(MAX_DISTANCE / max_exact)
                         * (N_BUCKETS - max_exact)).astype(jnp.int32)
    large = jnp.minimum(large, N_BUCKETS - 1)
    return jnp.where(n < max_exact, n, large)


def dilation_offsets():
    return jnp.asarray(np.stack([dil * np.arange(N_KEYS_A) for _, dil in DIL_CONFIGS]), dtype=jnp.int32)


def dilated_mixture_attention(q, k, v, bias_a):
    b, s = q.shape[:2]
    dist = dilation_offsets()
    g_idx = jnp.arange(N_DIL_GROUPS)[:, None, None]
    scale = HEAD_DIM ** -0.5

    def block(i):
        t = i * Q_BLOCK + jnp.arange(Q_BLOCK)
        key_pos = t[None, :, None] - dist[:, None, :]
        valid = key_pos >= 0
        kp = jnp.maximum(key_pos, 0)
        qb = lax.dynamic_slice_in_dim(q, i * Q_BLOCK, Q_BLOCK, axis=1)
        kg = k[:, kp, g_idx]
        vg = v[:, kp, g_idx]
        logits = jnp.einsum('bqghd,bgqjhd->bgqhj', qb, kg,
                            preferred_element_type=jnp.float32) * scale + bias_a[None, :, None]
        logits = jnp.where(valid[None, :, :, None, :], logits, NEG_INF)
        m = jnp.max(logits, axis=-1, keepdims=True)
        e = jnp.exp(logits - m)
        den = jnp.sum(e, axis=-1)
        o = jnp.einsum('bgqhj,bgqjhd->bgqhd', e, vg.astype(jnp.float32)) / den[..., None]
        lse = m[..., 0] + jnp.log(den)
        wts = jax.nn.softmax(lse, axis=1)
        return jnp.einsum('bgqh,bgqhd->bqhd', wts, o)

    out = lax.map(block, jnp.arange(s // Q_BLOCK))
    return out.transpose(1, 0, 2, 3, 4).reshape(b, s, HEADS_PER_DIL_GROUP, HEAD_DIM).astype(q.dtype)


def indexed_sparse_attention(q, k, v, q_idx, k_idx, w_idx, bias_b):
    b, s = q.shape[:2]
    topk = min(TOPK_MAX, s // 4)
    s_pos = jnp.arange(s)
    scale = HEAD_DIM ** -0.5
    idx_scale = IDX_DIM ** -0.5

    def block(i):
        t = i * Q_BLOCK + jnp.arange(Q_BLOCK)
        qi = lax.dynamic_slice_in_dim(q_idx, i * Q_BLOCK, Q_BLOCK, axis=1)
        wi = lax.dynamic_slice_in_dim(w_idx, i * Q_BLOCK, Q_BLOCK, axis=1)
        qb = lax.dynamic_slice_in_dim(q, i * Q_BLOCK, Q_BLOCK, axis=1)
        rel = jax.nn.relu(jnp.einsum('bqhd,bsd->bqsh', qi, k_idx,
                                     preferred_element_type=jnp.float32) * idx_scale)
        score = jnp.einsum('bqsh,bqh->bqs', rel, wi.astype(jnp.float32))
        causal = s_pos[None, :] <= t[:, None]
        score = jnp.where(causal[None], score, NEG_INF)
        _, sel = lax.top_k(score, topk)
        valid = sel <= t[None, :, None]
        kg = jax.vmap(lambda kk, ii: kk[ii])(k, sel)
        vg = jax.vmap(lambda vv, ii: vv[ii])(v, sel)
        logits = jnp.einsum('bqhd,bqkhd->bqkh', qb, kg, preferred_element_type=jnp.float32) * scale
        logits = logits + bias_b[t5_bucket(t[None, :, None] - sel)].astype(jnp.float32)
        logits = jnp.where(valid[..., None], logits, NEG_INF)
        p = jax.nn.softmax(logits, axis=2)
        return jnp.einsum('bqkh,bqkhd->bqhd', p, vg.astype(jnp.float32))

    out = lax.map(block, jnp.arange(s // Q_BLOCK))
    return out.transpose(1, 0, 2, 3, 4).reshape(b, s, HEADS_B, HEAD_DIM).astype(q.dtype)


def split_columns(proj):
    sizes = (3 * WIDTH_A, 3 * WIDTH_B, IDX_HEADS * IDX_DIM, IDX_DIM, IDX_HEADS, D_MODEL, D_MODEL)
    offs = np.cumsum(sizes)[:-1].tolist()
    return jnp.split(proj, offs, axis=-1)


def setup_inputs(seed: int = 0) -> dict:
    key = jax.random.key(seed)
    ks = jax.random.split(key, 14)
    f32 = jnp.float32
    nrm = lambda k, shape: jax.random.normal(k, shape, f32)
    return {
        "x": nrm(ks[0], (BATCH, SEQ, D_MODEL)),
        "norm_mix_g": 1.0 + 0.05 * nrm(ks[1], (DEPTH, D_MODEL)),
        "w_in": nrm(ks[2], (DEPTH, D_MODEL, D_IN)) * D_MODEL ** -0.5,
        "idx_k_norm_g": 1.0 + 0.05 * nrm(ks[3], (DEPTH, IDX_DIM)),
        "idx_k_norm_b": 0.01 * nrm(ks[4], (DEPTH, IDX_DIM)),
        "rel_bias_table": 0.5 * nrm(ks[5], (N_BUCKETS, N_ATTN_HEADS)),
        "w_proj_a": nrm(ks[6], (DEPTH, WIDTH_A_OUT, D_MODEL)) * WIDTH_A_OUT ** -0.5,
        "w_proj_b": nrm(ks[7], (DEPTH, WIDTH_B, D_MODEL)) * WIDTH_B ** -0.5,
        "w_out": nrm(ks[8], (DEPTH, D_MODEL, D_MODEL)) * D_MODEL ** -0.5,
        "norm_mlp_g": 1.0 + 0.05 * nrm(ks[9], (DEPTH, D_MODEL)),
        "w_mlp_up": nrm(ks[10], (DEPTH, D_MODEL, D_FF)) * D_MODEL ** -0.5,
        "w_mlp_down": nrm(ks[11], (DEPTH, D_FF, D_MODEL)) * D_FF ** -0.5,
        "norm_final_g": 1.0 + 0.05 * nrm(ks[12], (D_MODEL,)),
    }


def reference(x, norm_mix_g, w_in, idx_k_norm_g, idx_k_norm_b, rel_bias_table, w_proj_a, w_proj_b,
              w_out, norm_mlp_g, w_mlp_up, w_mlp_down, norm_final_g):
    b, s, _ = x.shape
    head_ids_a = jnp.arange(HEADS_A).reshape(N_DIL_GROUPS, HEADS_PER_DIL_GROUP)
    buckets_a = t5_bucket(dilation_offsets())
    bias_a = rel_bias_table[buckets_a[:, :, None], head_ids_a[:, None, :]]
    bias_a = bias_a.transpose(0, 2, 1).astype(jnp.float32)
    bias_b = rel_bias_table[:, HEADS_A:]

    for layer in range(DEPTH):
        h = rms_norm(x, norm_mix_g[layer])
        qkv_a, qkv_b, q_idx, k_idx, w_idx, gate_a, gate_b = split_columns(h @ w_in[layer])
        qkv_a = qkv_a.reshape(b, s, 3, N_DIL_GROUPS, HEADS_PER_DIL_GROUP, HEAD_DIM)
        qkv_b = qkv_b.reshape(b, s, 3, HEADS_B, HEAD_DIM)
        o_a = dilated_mixture_attention(qkv_a[:, :, 0], qkv_a[:, :, 1], qkv_a[:, :, 2], bias_a)
        q_idx = q_idx.reshape(b, s, IDX_HEADS, IDX_DIM)
        k_idx = layer_norm(k_idx, idx_k_norm_g[layer], idx_k_norm_b[layer])
        w_idx = w_idx * IDX_HEADS ** -0.5
        o_b = indexed_sparse_attention(qkv_b[:, :, 0], qkv_b[:, :, 1], qkv_b[:, :, 2],
                                       q_idx, k_idx, w_idx, bias_b)
        merged = (jax.nn.sigmoid(gate_a) * (o_a.reshape(b, s, WIDTH_A_OUT) @ w_proj_a[layer])
                  + jax.nn.sigmoid(gate_b) * (o_b.reshape(b, s, WIDTH_B) @ w_proj_b[layer]))
        x = x + merged @ w_out[layer]
        hm = rms_norm(x, norm_mlp_g[layer])
        x = x + jnp.square(jax.nn.relu(hm @ w_mlp_up[layer])) @ w_mlp_down[layer]
    return rms_norm(x, norm_final_g)
```

```python
import bisect
from contextlib import ExitStack

import numpy as np
import concourse.bass as bass
import concourse.mybir as mybir
from concourse.bass_utils import run_bass_kernel_spmd

F32 = mybir.dt.float32
BF16 = mybir.dt.bfloat16
AF = mybir.ActivationFunctionType
ALU = mybir.AluOpType
AX = mybir.AxisListType

S = 2048
D = 2048
DIN = 12880
DFF = 8192
NT = 16
KC = 16
EPS = 1e-6
SCALE = 128.0 ** -0.5
TOPK = 256
NIT = 14
NEG_RAW = -3000.0
NEGV = NEG_RAW / SCALE
NMV = -30000.0
MROW = 2559
WSTRIP = 2432
DILS = (1, 4, 16)
WINS = (128, 512, 2048)

C_QKVA = 0
C_QKVB = 4608
C_QIDX = 7680
C_KIDX = 8704
C_WIDX = 8768
C_GA = 8784
C_GB = 10832

FT_AQ = 0
FT_AK = 12
FT_BQ = 24
FT_BK = 32
FT_QI = 40
NFT = 48


class Buf:
    __slots__ = ("name", "wr", "rd")

    def __init__(self, name=""):
        self.name = name
        self.wr = {}
        self.rd = {}


class Chan:
    def __init__(self, sem, exact=True):
        self.sem = sem
        self.exact = exact
        self.ids = []


class Op:
    __slots__ = ("id", "eng", "fn", "deps", "chan", "ord", "inc", "incval")


def _ids(d):
    out = set()
    for k, v in d.items():
        if k == "dma":
            out.update(v)
        else:
            out.add(v)
    return out


class Prog:
    ENGS = ("pe", "act", "dve", "pool", "sp")

    def __init__(self):
        self.ops = []
        self.bar = set()
        self.bar_pending = {e: False for e in self.ENGS}
        self.last = {}
        self.dma_since = []
        self.out_chans = []

    def _addto(self, d, op):
        if op.chan is None:
            d[op.eng] = op.id
        else:
            d.setdefault("dma", []).append(op.id)

    def add(self, eng, fn, reads=(), writes=(), chan=None, nobar=False):
        op = Op()
        op.id = len(self.ops)
        op.eng = eng
        op.fn = fn
        op.chan = chan
        op.inc = False
        op.incval = 0
        op.ord = 0
        raw = set()
        oth = set()
        for b in reads:
            raw |= _ids(b.wr)
        for b in writes:
            oth |= _ids(b.wr)
            oth |= _ids(b.rd)
        if self.bar_pending[eng] and nobar not in (True, "in"):
            oth |= self.bar
            self.bar_pending[eng] = False
        final = set()
        for d in raw | oth:
            o = self.ops[d]
            if o.chan is not None or o.eng != eng:
                final.add(d)
            elif eng in ("act", "dve", "pool") and d in raw:
                final.add(d)
        op.deps = final
        for b in reads:
            if b not in writes:
                self._addto(b.rd, op)
        for b in writes:
            if b.rd:
                b.wr = {}
                b.rd = {}
            self._addto(b.wr, op)
        if chan is not None:
            chan.ids.append(op.id)
            op.ord = len(chan.ids)
            if nobar not in (True, "out"):
                self.dma_since.append(op.id)
        if nobar not in (True, "out"):
            self.last[eng] = op.id
        self.ops.append(op)
        return op

    def barrier(self):
        self.bar = set(self.last.values()) | set(self.dma_since)
        self.dma_since = []
        for e in self.ENGS:
            self.bar_pending[e] = True

    def emit(self, block, engsem):
        ops = self.ops
        for op in ops:
            for d in op.deps:
                if ops[d].chan is None:
                    ops[d].inc = True
        cnt = {e: 0 for e in self.ENGS}
        for op in ops:
            if op.chan is None and op.inc:
                cnt[op.eng] += 1
                op.incval = cnt[op.eng]
        by_eng = {e: [op for op in ops if op.eng == e] for e in self.ENGS}
        out_chans = self.out_chans

        def run(h, eng):
            waited = {}
            for op in by_eng[eng]:
                need = {}
                for d in op.deps:
                    o = ops[d]
                    if o.chan is not None:
                        key = ("c", id(o.chan))
                        sem = o.chan.sem
                        if o.chan.exact:
                            val = 16 * o.ord
                        else:
                            val = 16 * bisect.bisect_left(o.chan.ids, op.id)
                    else:
                        key = ("e", o.eng)
                        sem = engsem[o.eng]
                        val = o.incval
                    if key not in need or need[key][1] < val:
                        need[key] = (sem, val)
                for key, (sem, val) in need.items():
                    if waited.get(key, 0) < val:
                        h.wait_ge(sem, val)
                        waited[key] = val
                ins = op.fn(h)
                if op.chan is not None:
                    ins.then_inc(op.chan.sem, 16)
                elif op.inc:
                    ins.then_inc(engsem[eng], 1)
            if eng == "sp":
                for ch in out_chans:
                    h.wait_ge(ch.sem, 16 * len(ch.ids))

        @block.tensor
        def _(h):
            run(h, "pe")

        @block.scalar
        def _(h):
            run(h, "act")

        @block.vector
        def _(h):
            run(h, "dve")

        @block.gpsimd
        def _(h):
            run(h, "pool")

        @block.sync
        def _(h):
            run(h, "sp")


def _t5_bucket_np(n):
    n = np.maximum(n, 0)
    nf = np.maximum(n, 1).astype(np.float32)
    large = 16 + (np.log(nf / np.float32(16)) / np.float32(np.log(2048 / 16)) * np.float32(16)).astype(np.int32)
    large = np.minimum(large, 31)
    return np.where(n < 16, n, large)


def _onehot_tables():
    m = np.arange(MROW)
    n = m - 511
    bk = _t5_bucket_np(n)
    oh = np.zeros((4, 33, MROW), np.float32)
    for v in range(4):
        if v < 3:
            dil, win = DILS[v], WINS[v]
            valid = (n >= 0) & (n % dil == 0) & (n <= win)
        else:
            valid = n >= 0
        oh[v, bk[valid], m[valid]] = 1.0
        oh[v, 32, m[~valid]] = 1.0
    return oh


def build_program(debug=False, phase_limit=99):
    nc = bass.Bass("TRN2", target_bir_lowering=False)
    dram = lambda name, shape, dt, kind="ExternalInput": nc.dram_tensor(name, shape, dt, kind=kind)
    x_d = dram("x", [S, D], F32).ap()
    w_in_d = dram("w_in", [D, DIN], F32).ap()
    wpa_d = dram("w_proj_a", [512, D], F32).ap()
    wpb_d = dram("w_proj_b", [1024, D], F32).ap()
    wout_d = dram("w_out", [D, D], F32).ap()
    wup_d = dram("w_mlp_up", [D, DFF], F32).ap()
    wdn_d = dram("w_mlp_down", [DFF, D], F32).ap()
    gmix_d = dram("norm_mix_g", [D], F32).ap()
    gmlp_d = dram("norm_mlp_g", [D], F32).ap()
    gfin_d = dram("norm_final_g", [D], F32).ap()
    ikg_d = dram("idx_k_norm_g", [64], F32).ap()
    ikb_d = dram("idx_k_norm_b", [64], F32).ap()
    tab_d = dram("rel_bias_table", [32, 20], F32).ap()
    oh_d = dram("onehot", [4, 33, MROW], F32).ap()
    pw2_d = dram("pow2", [NIT], F32).ap()
    out_d = dram("out", [S, D], F32, kind="ExternalOutput").ap()
    skind = "ExternalOutput" if debug else "Internal"
    qk_t = dram("qk_scr", [NFT, 128, S], BF16, kind=skind)
    v_t = dram("v_scr", [5, S, 512], BF16, kind=skind)
    gate_t = dram("gate_scr", [32, 128, S], BF16, kind=skind)
    r_t = dram("r_scr", [20, MROW], BF16, kind=skind)
    nm_t = dram("nm_scr", [NT, 128, S], BF16, kind=skind)
    x1_t = dram("x1_scr", [S, D], F32, kind=skind)
    ob_t = dram("o_scr", [12, 128, S], BF16, kind=skind)
    wupb_t = dram("wupb_scr", [D, DFF], BF16, kind="Internal")
    wdnb_t = dram("wdnb_scr", [DFF, D], BF16, kind="Internal")
    wupb_s, wdnb_s = wupb_t.ap(), wdnb_t.ap()
    qk_s, v_s, gate_s, r_s, nm_s, x1_s, o_s = (t.ap() for t in (qk_t, v_t, gate_t, r_t, nm_t, x1_t, ob_t))

    P = Prog()
    es = ExitStack()
    with es:
        ARENA_W = 200 * 256
        arena = es.enter_context(nc.sbuf_tensor("arena", [128, ARENA_W], F32))
        identF = es.enter_context(nc.sbuf_tensor("identF", [128, 128], F32))
        identB = es.enter_context(nc.sbuf_tensor("identB", [128, 128], BF16))
        antiJ = es.enter_context(nc.sbuf_tensor("antiJ", [128, 128], BF16))
        onesB = es.enter_context(nc.sbuf_tensor("onesB", [128, 128], BF16))
        onesF = es.enter_context(nc.sbuf_tensor("onesF", [128, 128], F32))
        SM = es.enter_context(nc.sbuf_tensor("SM", [128, 1024], F32))
        ps = [es.enter_context(nc.psum_tensor(f"ps{i}", [128, 512], F32)) for i in range(8)]
        engsem = {e: es.enter_context(nc.semaphore(f"sem_{e}")) for e in ("pe", "act", "dve", "pool")}

        def new_chan(name, exact=True):
            return Chan(es.enter_context(nc.semaphore(name)), exact)

        block = es.enter_context(nc.Block())

        PS = [Buf(f"ps{i}") for i in range(8)]
        B_const = Buf("const")
        ch_misc = new_chan("ch_misc", exact=False)

        def carve(off_kb, shape, dt):
            n = int(np.prod(shape))
            esz = 4 if dt == F32 else 2
            nbytes = n * esz
            w0 = int(off_kb * 256)
            nw = (nbytes + 3) // 4
            assert w0 + nw <= ARENA_W, (off_kb, shape)
            a = arena[:, w0:w0 + nw]
            if dt != F32:
                a = a.bitcast(dt)[:, 0:n]
            if len(shape) == 2:
                a = a.rearrange("p (a b) -> p a b", a=shape[0])
            elif len(shape) == 3:
                a = a.rearrange("p (a b c) -> p a b c", a=shape[0], b=shape[1])
            return a

        c_ssq = 0
        c_rt = 16
        c_rstd = 32
        c_eps = 48
        c_ikg = 50
        c_ikb = 51
        c_s1 = 64
        c_negmu = 80
        c_s2 = 96
        c_sd = 112
        c_rs = 128
        c_lo = 144
        c_w0 = 160
        c_mid = 176
        c_cnt = 192
        c_dd = 208
        c_ssq1 = 224
        c_ssq2 = 288
        c_t1 = 352
        c_t2 = 368
        c_pw2 = 384
        c_steps = 416
        c_hi = 800
        c_gT = 820

        def mk_consts():
            for t_, pat, base in ((identF, [[-1, 128]], 0), (identB, [[-1, 128]], 0), (antiJ, [[1, 128]], -127)):
                bt = Buf("c_tmp")
                P.add("pool", lambda h, t_=t_: h.memset(t_[:], 1.0), writes=[bt])
                P.add("pool", lambda h, t_=t_, pat=pat, base=base: h.affine_select(
                    out=t_[:], in_=t_[:], pattern=pat, compare_op=ALU.is_equal, fill=0.0, base=base, channel_multiplier=1),
                    reads=[bt], writes=[bt, B_const])
            P.add("pool", lambda h: h.memset(onesB[:], 1.0), writes=[B_const])
            P.add("pool", lambda h: h.memset(onesF[:], 1.0), writes=[B_const])
            P.add("pool", lambda h: h.memset(SM[:, c_eps:c_eps + 1], EPS), writes=[B_const])
            P.add("sp", lambda h: h.dma_start(out=SM[:, c_pw2:c_pw2 + NIT], in_=pw2_d.partition_broadcast(128)),
                  writes=[B_const], chan=ch_misc)
            P.add("sp", lambda h: h.dma_start(out=SM[0:64, c_ikg:c_ikg + 1], in_=ikg_d.rearrange("(p o) -> p o", o=1)),
                  writes=[B_const], chan=ch_misc)
            P.add("sp", lambda h: h.dma_start(out=SM[64:128, c_ikg:c_ikg + 1], in_=ikg_d.rearrange("(p o) -> p o", o=1)),
                  writes=[B_const], chan=ch_misc)
            P.add("sp", lambda h: h.dma_start(out=SM[0:64, c_ikb:c_ikb + 1], in_=ikb_d.rearrange("(p o) -> p o", o=1)),
                  writes=[B_const], chan=ch_misc)
            P.add("sp", lambda h: h.dma_start(out=SM[64:128, c_ikb:c_ikb + 1], in_=ikb_d.rearrange("(p o) -> p o", o=1)),
                  writes=[B_const], chan=ch_misc)

        mk_consts()

        def mk_rrows():
            tabX = carve(0, [20], F32)
            ohs = carve(1, [MROW], F32)
            rsb = carve(12, [MROW], BF16)
            B_tab, B_oh, B_rsb = Buf("tab"), Buf("oh"), Buf("rsb")
            P.add("pool", lambda h: h.memset(tabX[32:33, :], NEG_RAW), writes=[B_tab])
            P.add("sp", lambda h: h.dma_start(out=tabX[0:32, :], in_=tab_d), writes=[B_tab], chan=ch_misc)
            for v in range(4):
                nh = 4 if v < 3 else 8
                c0 = v * 4
                P.add("sp", lambda h, v=v: h.dma_start(out=ohs[0:33, :], in_=oh_d[v]), writes=[B_oh], chan=ch_misc)
                for k in range(5):
                    w = min(512, MROW - 512 * k)
                    pb = k % 2
                    P.add("pe", lambda h, k=k, w=w, pb=pb, c0=c0, nh=nh: h.matmul(
                        ps[pb][0:nh, 0:w], lhsT=tabX[0:33, c0:c0 + nh], rhs=ohs[0:33, 512 * k:512 * k + w],
                        start=True, stop=True), reads=[B_tab, B_oh, B_const], writes=[PS[pb]])
                    P.add("act", lambda h, k=k, w=w, pb=pb, nh=nh: h.activation(
                        out=rsb[0:nh, 512 * k:512 * k + w], in_=ps[pb][0:nh, 0:w], func=AF.Copy, scale=1.0 / SCALE),
                        reads=[PS[pb]], writes=[B_rsb])
                P.add("sp", lambda h, c0=c0, nh=nh: h.dma_start(out=r_s[c0:c0 + nh, :], in_=rsb[0:nh, :]),
                      reads=[B_rsb], writes=[B_rscr], chan=ch_misc)

        B_rscr = Buf("r_scr")
        mk_rrows()
        P.barrier()

        kw = carve(0, [NT, 80], F32)
        hT = carve(8, [KC, S], BF16)
        wsl = [carve(72 + 16 * i, [16, 512], BF16) for i in range(3)]
        XT = [carve(120 + 8 * i, [D], F32) for i in range(2)]
        hbs = [carve(136, [D], F32), carve(176, [D], F32)]
        junk = carve(144, [D], BF16)
        gbc = carve(148, [D], F32)
        stFM = [carve(156 + 4 * i, [S], BF16) for i in range(3)]
        stTM = [carve(168 + 4 * i, [4, 512], BF16) for i in range(2)]
        B_kw = Buf("kw")
        B_hT = [Buf(f"hT{i}") for i in range(NT)]
        B_wsl = [Buf(f"wsl{i}") for i in range(3)]
        ch_wsl = [new_chan(f"ch_wsl{i}") for i in range(3)]
        B_XT = [Buf(f"XT{i}") for i in range(2)]
        ch_XT = [new_chan(f"ch_XT{i}") for i in range(2)]
        B_hbs, B_junkA, B_junkD, B_gbc = [Buf("hb0"), Buf("hb1")], Buf("junkA"), Buf("junkD"), Buf("gbc")
        B_stFM = [Buf(f"stFM{i}") for i in range(3)]
        ch_stFM = [new_chan(f"ch_stFM{i}") for i in range(3)]
        B_stTM = [Buf(f"stTM{i}") for i in range(2)]
        ch_stTM = [new_chan(f"ch_stTM{i}") for i in range(2)]
        B_sm = Buf("sm")

        evac_rr = [0]

        def evac_copy(out_ap, in_ap, reads, writes, scale=None):
            evac_rr[0] ^= 1
            if evac_rr[0]:
                if scale is None:
                    P.add("act", lambda h: h.activation(out=out_ap, in_=in_ap, func=AF.Copy), reads=reads, writes=writes)
                else:
                    fn_ = AF.Copy if isinstance(scale, float) else AF.Identity
                    P.add("act", lambda h: h.activation(out=out_ap, in_=in_ap, func=fn_, scale=scale),
                          reads=reads, writes=writes)
            else:
                if scale is None:
                    P.add("dve", lambda h: h.tensor_copy(out=out_ap, in_=in_ap), reads=reads, writes=writes)
                else:
                    P.add("dve", lambda h: h.tensor_scalar(out=out_ap, in0=in_ap, scalar1=scale, scalar2=None,
                                                           op0=ALU.mult), reads=reads, writes=writes)

        def rstd_ops(ssq_col, n, rt_col, rstd_col, inv_n, bsm=None):
            bsm = bsm or B_sm
            P.add("act", lambda h: h.activation(out=SM[:, rt_col:rt_col + n], in_=SM[:, ssq_col:ssq_col + n],
                                                func=AF.Sqrt, scale=inv_n, bias=SM[:, c_eps:c_eps + 1]),
                  reads=[bsm, B_const], writes=[bsm])
            P.add("dve", lambda h: h.reciprocal(out=SM[:, rstd_col:rstd_col + n], in_=SM[:, rt_col:rt_col + n]),
                  reads=[bsm], writes=[bsm])

        P.add("sp", lambda h: h.dma_start(out=gbc, in_=gmix_d.partition_broadcast(128)), writes=[B_gbc], chan=ch_misc)
        B_xld = Buf("xld")

        def p1_stage_a(i):
            sl = i % 2
            P.add("sp", lambda h: h.dma_start(out=XT[sl], in_=x_d[i * 128:(i + 1) * 128, :]),
                  writes=[B_XT[sl]] + ([B_xld] if i == 11 else []), chan=ch_XT[sl])
            bsm_i = Buf(f"smP1_{i}")
            hb = hbs[sl]
            P.add("act", lambda h: h.activation(out=junk, in_=XT[sl], func=AF.Square,
                                                accum_out=SM[:, c_ssq + i:c_ssq + i + 1]),
                  reads=[B_XT[sl]], writes=[B_junkA, bsm_i])
            rstd_ops(c_ssq + i, 1, c_rt + i, c_rstd + i, 1.0 / D, bsm=bsm_i)
            P.add("dve", lambda h: h.scalar_tensor_tensor(
                out=hb, in0=XT[sl], scalar=SM[:, c_rstd + i:c_rstd + i + 1], in1=gbc, op0=ALU.mult, op1=ALU.mult),
                reads=[B_XT[sl], bsm_i, B_gbc], writes=[B_hbs[sl]])

        def p1_stage_b(i):
            sl = i % 2
            hb = hbs[sl]
            for q in range(4):
                pb = q % 4
                def tr(h, q=q, pb=pb):
                    for j in range(4):
                        kc = 4 * q + j
                        ins = h.transpose(out=ps[pb][:, j * 128:(j + 1) * 128], in_=hb[:, kc * 128:(kc + 1) * 128],
                                          identity=identF[:])
                    return ins
                P.add("pe", tr, reads=[B_hbs[sl], B_const], writes=[PS[pb]])
                evac_copy(hT[:, 4 * q:4 * q + 4, i * 128:(i + 1) * 128],
                          ps[pb][:, :].rearrange("p (a b) -> p a b", a=4), [PS[pb]], [B_hT[i]])

        p1_stage_a(0)
        for i in range(NT):
            if i + 1 < NT:
                p1_stage_a(i + 1)
            p1_stage_b(i)

        w_in_v = w_in_d.rearrange("(k p) n -> p k n", p=128)
        slab_ctr = [0]
        sweep_on = [False]

        def load_slab(src3, slots, ncols=512, nk=16, reads=(), nobar=False):
            s = slab_ctr[0] % 3
            slab_ctr[0] += 1
            dst = slots[s][:, 0:nk, 0:ncols]
            if slab_ctr[0] == 1:
                reads = list(reads) + [B_xld]
            P.add("pool", lambda h: h.dma_start(out=dst, in_=src3), reads=list(reads), writes=[B_wsl[s]], chan=ch_wsl[s],
                  nobar=nobar)
            if sweep_on[0]:
                if slab_ctr[0] > 2:
                    emit_conv(2 if slab_ctr[0] <= 10 else 1)
            return s

        fm_ctr = [0]
        psr = [0]

        def next_ps(lo=0, n=4):
            psr[0] = (psr[0] + 1) % n
            return lo + psr[0]

        def proj_fm(c0, ft_base, gate=False, dst=None):
            s = load_slab(w_in_v[:, :, c0:c0 + 512], wsl)
            W = wsl[s]
            for ft in range(4):
                st = fm_ctr[0] % 3
                fm_ctr[0] += 1
                for tc in range(4):
                    pb = next_ps()
                    def mm(h, ft=ft, tc=tc, pb=pb, W=W):
                        for kc in range(KC):
                            ins = h.matmul(ps[pb][:, :], lhsT=W[:, kc, ft * 128:(ft + 1) * 128],
                                           rhs=hT[:, kc, tc * 512:(tc + 1) * 512], start=(kc == 0), stop=(kc == KC - 1))
                        return ins
                    P.add("pe", mm, reads=[B_wsl[s]] + B_hT[4 * tc:4 * tc + 4], writes=[PS[pb]])
                    o_ap = stFM[st][:, tc * 512:(tc + 1) * 512]
                    if gate:
                        P.add("act", lambda h, o_ap=o_ap, pb=pb: h.activation(out=o_ap, in_=ps[pb][:, :], func=AF.Sigmoid),
                              reads=[PS[pb]], writes=[B_stFM[st]])
                    else:
                        evac_copy(o_ap, ps[pb][:, :], [PS[pb]], [B_stFM[st]])
                tgt = (dst if dst is not None else qk_s)[ft_base + ft]
                P.add("sp", lambda h, st=st, tgt=tgt: h.dma_start(out=tgt, in_=stFM[st]),
                      reads=[B_stFM[st]], writes=[B_scr], chan=ch_stFM[st])

        tm_ctr = [0]

        def proj_tm(c0, vslab):
            s = load_slab(w_in_v[:, :, c0:c0 + 512], wsl)
            W = wsl[s]
            for tg in range(4):
                st = tm_ctr[0] % 2
                tm_ctr[0] += 1
                for tl in range(4):
                    tt = 4 * tg + tl
                    pb = next_ps()
                    def mm(h, tt=tt, pb=pb, W=W):
                        for kc in range(KC):
                            ins = h.matmul(ps[pb][:, :], lhsT=hT[:, kc, tt * 128:(tt + 1) * 128], rhs=W[:, kc, :],
                                           start=(kc == 0), stop=(kc == KC - 1))
                        return ins
                    P.add("pe", mm, reads=[B_wsl[s], B_hT[tt]], writes=[PS[pb]])
                    evac_copy(stTM[st][:, tl, :], ps[pb][:, :], [PS[pb]], [B_stTM[st]])
                tgt = v_s[vslab, tg * 512:(tg + 1) * 512, :].rearrange("(a p) c -> p a c", p=128)
                P.add("sp", lambda h, st=st, tgt=tgt: h.dma_start(out=tgt, in_=stTM[st]),
                      reads=[B_stTM[st]], writes=[B_scr], chan=ch_stTM[st])

        B_wbf = Buf("wbf")
        ch_conv = [new_chan(f"ch_conv{i}") for i in range(4)]
        B_cv = [Buf(f"cv{i}") for i in range(4)]
        conv_list = []
        for kc in range(KC):
            conv_list.append((wupb_s[kc * 128:(kc + 1) * 128, :], wup_d[kc * 128:(kc + 1) * 128, :]))
        for kc in range(16):
            conv_list.append((wdnb_s[kc * 512:(kc + 1) * 512, :].rearrange("(p a) n -> p (a n)", a=4),
                              wdn_d[kc * 512:(kc + 1) * 512, :].rearrange("(p a) n -> p (a n)", a=4)))
        conv_ctr = [0]

        def emit_conv(n):
            for _ in range(n):
                if not conv_list:
                    return
                o_ap, i_ap = conv_list.pop(0)
                k = conv_ctr[0] % 4
                conv_ctr[0] += 1
                P.add("pool", lambda h, o_ap=o_ap, i_ap=i_ap: h.dma_start(out=o_ap, in_=i_ap),
                      writes=[B_cv[k]], chan=ch_conv[k])

        B_scr = Buf("scr")

        if phase_limit >= 2:
            sweep_on[0] = True
            for g in range(3):
                proj_fm(C_QKVA + (0 * 3 + g) * 512, FT_AQ + 4 * g)
                proj_fm(C_QKVA + (1 * 3 + g) * 512, FT_AK + 4 * g)
                proj_tm(C_QKVA + (2 * 3 + g) * 512, g)
            for hf in range(2):
                proj_fm(C_QKVB + hf * 512, FT_BQ + 4 * hf)
                proj_fm(C_QKVB + 1024 + hf * 512, FT_BK + 4 * hf)
                proj_tm(C_QKVB + 2048 + hf * 512, 3 + hf)
            for hf in range(2):
                proj_fm(C_QIDX + hf * 512, FT_QI + 4 * hf)
            s = load_slab(w_in_v[:, :, C_KIDX:C_KIDX + 80], wsl, ncols=80)
            W = wsl[s]
            for tt in range(NT):
                pb = next_ps()
                def mm(h, tt=tt, pb=pb, W=W):
                    for kc in range(KC):
                        ins = h.matmul(ps[pb][:, 0:80], lhsT=hT[:, kc, tt * 128:(tt + 1) * 128], rhs=W[:, kc, 0:80],
                                       start=(kc == 0), stop=(kc == KC - 1))
                    return ins
                P.add("pe", mm, reads=[B_wsl[s], B_hT[tt]], writes=[PS[pb]])
                evac_copy(kw[:, tt, :], ps[pb][:, 0:80], [PS[pb]], [B_kw])
            for gi in range(8):
                proj_fm(C_GA + gi * 512, 4 * gi, gate=True, dst=gate_s)
            while conv_list:
                emit_conv(1)
            sweep_on[0] = False
        P.barrier()

        if phase_limit >= 3:
            offs = [128 * (i * (i + 1) // 2) for i in range(NT + 1)]
            scores = carve(8, [offs[NT]], F32)
            kiT = carve(76, [S], BF16)
            kn2 = carve(80, [NT, 128], F32)
            qiT = [carve(88 + 2 * i, [8, 128], BF16) for i in range(2)]
            rbuf = [carve(92 + 2 * i, [512], F32) for i in range(3)] + [carve(112, [512], F32)]
            junk2 = carve(98, [S], BF16)
            nmst = [carve(102 + 4 * i, [S], BF16) for i in range(2)]
            xc = carve(110, [64], F32)
            wsc = carve(111, [NT, 16], F32)
            B_scores = [Buf(f"sc{i}") for i in range(NT)]
            B_kiT, B_kn2, B_xc, B_wsc, B_junk2 = Buf("kiT"), Buf("kn2"), Buf("xc"), Buf("wsc"), Buf("junk2")
            B_qiT = [Buf(f"qiT{i}") for i in range(2)]
            ch_qiT = [new_chan(f"ch_qiT{i}") for i in range(2)]
            B_rbuf = [Buf(f"rbuf{i}") for i in range(4)]
            rbufP = [carve(114 + 2 * i, [512], F32) for i in range(4)]
            tmpP = carve(122, [512], F32)
            accP = [carve(124 + 2 * i, [512], F32) for i in range(2)]
            B_rbufP = [Buf(f"rbufP{i}") for i in range(4)]
            B_tmpP = Buf("tmpP")
            B_accP = [Buf("accP0"), Buf("accP1")]
            B_nmst = [Buf(f"nmst{i}") for i in range(2)]
            ch_nmst = [new_chan(f"ch_nmst{i}") for i in range(2)]
            B_nmscr = Buf("nmscr")

            for i in range(NT):
                kv = kw[:, i, 0:64]
                P.add("act", lambda h, i=i, kv=kv: h.activation(out=xc, in_=kv, func=AF.Identity,
                                                                accum_out=SM[:, c_s1 + i:c_s1 + i + 1]),
                      reads=[B_kw], writes=[B_xc, B_sm])
                P.add("dve", lambda h, i=i: h.tensor_scalar(out=SM[:, c_negmu + i:c_negmu + i + 1],
                                                            in0=SM[:, c_s1 + i:c_s1 + i + 1], scalar1=-1.0 / 64,
                                                            scalar2=None, op0=ALU.mult), reads=[B_sm], writes=[B_sm])
                P.add("act", lambda h, i=i, kv=kv: h.activation(out=kn2[:, i, 0:64], in_=kv, func=AF.Identity,
                                                                bias=SM[:, c_negmu + i:c_negmu + i + 1], scale=1.0),
                      reads=[B_kw, B_sm], writes=[B_kn2])
                P.add("act", lambda h, i=i: h.activation(out=xc, in_=kn2[:, i, 0:64], func=AF.Square,
                                                         accum_out=SM[:, c_s2 + i:c_s2 + i + 1]),
                      reads=[B_kn2], writes=[B_xc, B_sm])
            rstd_ops(c_s2, NT, c_sd, c_rs, 1.0 / 64)
            for i in range(NT):
                P.add("dve", lambda h, i=i: h.tensor_scalar(out=kn2[:, i, 64:128], in0=kn2[:, i, 0:64],
                                                            scalar1=SM[:, c_rs + i:c_rs + i + 1], scalar2=None,
                                                            op0=ALU.mult), reads=[B_kn2, B_sm], writes=[B_kn2])
                P.add("dve", lambda h, i=i: h.tensor_copy(out=kn2[:, i, 0:64], in_=kn2[:, i, 64:128]),
                      reads=[B_kn2], writes=[B_kn2])
                pb = next_ps()
                P.add("pe", lambda h, i=i, pb=pb: h.transpose(out=ps[pb][:, 0:128], in_=kn2[:, i, :], identity=identF[:]),
                      reads=[B_kn2, B_const], writes=[PS[pb]])
                P.add("act", lambda h, i=i, pb=pb: h.activation(out=kiT[:, i * 128:(i + 1) * 128], in_=ps[pb][:, 0:128],
                                                                func=AF.Identity, scale=SM[:, c_ikg:c_ikg + 1],
                                                                bias=SM[:, c_ikb:c_ikb + 1]),
                      reads=[PS[pb], B_const], writes=[B_kiT])
            P.add("dve", lambda h: h.tensor_scalar(out=wsc, in0=kw[:, :, 64:80], scalar1=2.0 ** -5, scalar2=None,
                                                   op0=ALU.mult), reads=[B_kw], writes=[B_wsc])

            P.add("pool", lambda h: h.memset(nmst[0][:, 0:256], 0.0), writes=[B_nmst[0]])
            for i in range(2):
                P.add("sp", lambda h, i=i: h.dma_start(out=nm_s[i, :, 0:128 * (i + 1)], in_=nmst[0][:, 0:128 * (i + 1)]),
                      reads=[B_nmst[0]], writes=[B_nmscr], chan=ch_nmst[0])

            qi_src = qk_s[FT_QI:FT_QI + 8].rearrange("f p c -> p f c")
            for i in range(2, NT):
                sl = i % 2
                L = 128 * (i + 1)
                P.add("sp", lambda h, i=i, sl=sl: h.dma_start(out=qiT[sl], in_=qi_src[:, :, i * 128:(i + 1) * 128]),
                      writes=[B_qiT[sl]], chan=ch_qiT[sl])
                nch = (L + 511) // 512
                for c in range(nch):
                    wc = min(512, L - 512 * c)
                    pacc0 = 4 + 2 * (c % 2)
                    accp = accP[c % 2]
                    B_accp = B_accP[c % 2]
                    n_dve = 0
                    for hh in (0, 1, 2, 12, 3, 4, 5, 13, 6, 7, 8, 14, 9, 10, 11, 15):
                        po = (hh % 2) * 64
                        pb = next_ps(0, 3)
                        on_pool = hh >= 12
                        if on_pool:
                            rtile, B_rt = rbufP[hh - 12], B_rbufP[hh - 12]
                        else:
                            rtile, B_rt = rbuf[n_dve % 3], B_rbuf[n_dve % 3]
                        P.add("pe", lambda h, hh=hh, po=po, pb=pb, sl=sl, c=c, wc=wc: h.matmul(
                            ps[pb][:, 0:wc], lhsT=qiT[sl][po:po + 64, hh // 2, :],
                            rhs=kiT[po:po + 64, c * 512:c * 512 + wc], start=True, stop=True),
                            reads=[B_qiT[sl], B_kiT], writes=[PS[pb]])
                        P.add("act", lambda h, pb=pb, rtile=rtile, wc=wc: h.activation(out=rtile[:, 0:wc], in_=ps[pb][:, 0:wc],
                                                                                       func=AF.Relu),
                              reads=[PS[pb]], writes=[B_rt])
                        wcol = wsc[:, i, hh:hh + 1]
                        if on_pool:
                            if hh == 12:
                                P.add("pool", lambda h, rtile=rtile, wc=wc, wcol=wcol, accp=accp: h.tensor_scalar(
                                    out=accp[:, 0:wc], in0=rtile[:, 0:wc], scalar1=wcol, scalar2=0.0, op0=ALU.mult,
                                    op1=ALU.add), reads=[B_rt, B_wsc], writes=[B_accp])
                            else:
                                P.add("pool", lambda h, rtile=rtile, wc=wc, wcol=wcol: h.tensor_scalar(
                                    out=tmpP[:, 0:wc], in0=rtile[:, 0:wc], scalar1=wcol, scalar2=0.0, op0=ALU.mult,
                                    op1=ALU.add), reads=[B_rt, B_wsc], writes=[B_tmpP])
                                P.add("pool", lambda h, wc=wc, accp=accp: h.tensor_tensor(
                                    out=accp[:, 0:wc], in0=accp[:, 0:wc], in1=tmpP[:, 0:wc], op=ALU.add),
                                    reads=[B_tmpP, B_accp], writes=[B_accp])
                            continue
                        pacc = pacc0 + (n_dve % 2)
                        if n_dve < 2:
                            P.add("dve", lambda h, rtile=rtile, wc=wc, pacc=pacc, wcol=wcol: h.tensor_scalar(
                                out=ps[pacc][:, 0:wc], in0=rtile[:, 0:wc], scalar1=wcol, scalar2=None, op0=ALU.mult),
                                reads=[B_rt, B_wsc], writes=[PS[pacc]])
                        else:
                            P.add("dve", lambda h, rtile=rtile, wc=wc, pacc=pacc, wcol=wcol: h.scalar_tensor_tensor(
                                out=ps[pacc][:, 0:wc], in0=rtile[:, 0:wc], scalar=wcol, in1=ps[pacc][:, 0:wc],
                                op0=ALU.mult, op1=ALU.add), reads=[B_rt, B_wsc, PS[pacc]], writes=[PS[pacc]])
                        n_dve += 1
                    P.add("act", lambda h, wc=wc, pacc0=pacc0: h.activation(
                        out=rbuf[3][:, 0:wc], in_=ps[pacc0 + 1][:, 0:wc], func=AF.Copy),
                        reads=[PS[pacc0 + 1]], writes=[B_rbuf[3]])
                    sc_ap = scores[:, offs[i] + 512 * c:offs[i] + 512 * c + wc]
                    P.add("dve", lambda h, sc_ap=sc_ap, wc=wc, pacc0=pacc0: h.tensor_tensor(
                        out=sc_ap, in0=ps[pacc0][:, 0:wc], in1=rbuf[3][:, 0:wc], op=ALU.add),
                        reads=[PS[pacc0], B_rbuf[3]], writes=[B_scores[i]])
                    P.add("pool", lambda h, sc_ap=sc_ap, wc=wc, accp=accp: h.tensor_tensor(
                        out=sc_ap, in0=sc_ap, in1=accp[:, 0:wc], op=ALU.add),
                        reads=[B_scores[i], B_accp], writes=[B_scores[i]])
                P.add("pool", lambda h, i=i: h.affine_select(
                    out=scores[:, offs[i] + 128 * i:offs[i] + 128 * (i + 1)],
                    in_=scores[:, offs[i] + 128 * i:offs[i] + 128 * (i + 1)], pattern=[[-1, 128]],
                    compare_op=ALU.is_ge, fill=-1e30, base=0, channel_multiplier=1),
                    reads=[B_scores[i]], writes=[B_scores[i]])
                P.add("dve", lambda h, i=i, L=L: h.tensor_reduce(out=SM[:, c_hi + i:c_hi + i + 1],
                                                                 in_=scores[:, offs[i]:offs[i] + L], axis=AX.X, op=ALU.max),
                      reads=[B_scores[i]], writes=[B_sm])
                P.add("dve", lambda h, i=i: h.tensor_reduce(out=SM[:, c_lo + i:c_lo + i + 1],
                                                            in_=scores[:, offs[i]:offs[i] + 128 * i], axis=AX.X, op=ALU.min),
                      reads=[B_scores[i]], writes=[B_sm])
        P.barrier()

        OA = [carve(140 + 4 * i, [S], BF16) for i in range(4)]
        OB = [carve(156 + 4 * i, [S], BF16) for i in range(8)]
        B_OA = [Buf(f"OA{i}") for i in range(4)]
        B_OB = [Buf(f"OB{i}") for i in range(8)]

        def bisect_gen():
            junk2 = carve(76, [S], BF16)
            nmst = [carve(80 + 4 * i, [S], BF16) for i in range(2)]
            B_junk2 = Buf("junk2b")
            B_nmst = [Buf(f"nmstb{i}") for i in range(2)]
            T0, T1 = 2, NT
            P.add("dve", lambda h: h.tensor_tensor(out=SM[:, c_w0 + T0:c_w0 + T1], in0=SM[:, c_hi + T0:c_hi + T1],
                                                   in1=SM[:, c_lo + T0:c_lo + T1], op=ALU.subtract),
                  reads=[B_sm], writes=[B_sm])
            P.add("dve", lambda h: h.tensor_scalar(out=SM[:, c_w0 + T0:c_w0 + T1], in0=SM[:, c_w0 + T0:c_w0 + T1],
                                                   scalar1=1.001, scalar2=1e-6, op0=ALU.mult, op1=ALU.add),
                  reads=[B_sm], writes=[B_sm])
            for i in range(T0, T1):
                P.add("dve", lambda h, i=i: h.tensor_scalar(out=SM[:, c_steps + i * NIT:c_steps + (i + 1) * NIT],
                                                            in0=SM[:, c_pw2:c_pw2 + NIT], scalar1=SM[:, c_w0 + i:c_w0 + i + 1],
                                                            scalar2=None, op0=ALU.mult), reads=[B_sm, B_const], writes=[B_sm])
            yield 2.0
            steps_v = SM[:, c_steps:c_steps + NT * NIT].rearrange("p (a b) -> p a b", b=NIT)
            for k in range(NIT):
                stepk = steps_v[:, T0:T1, k]
                P.add("dve", lambda h, stepk=stepk: h.tensor_tensor(out=SM[:, c_mid + T0:c_mid + T1],
                                                                    in0=SM[:, c_lo + T0:c_lo + T1], in1=stepk, op=ALU.add),
                      reads=[B_sm], writes=[B_sm])
                for i in range(T0, T1):
                    L = 128 * (i + 1)
                    P.add("dve", lambda h, i=i, L=L: h.tensor_scalar(
                        out=junk2[:, 0:L], in0=scores[:, offs[i]:offs[i] + L], scalar1=SM[:, c_mid + i:c_mid + i + 1],
                        scalar2=None, op0=ALU.is_ge, op1=ALU.add, accum_out=SM[:, c_cnt + i:c_cnt + i + 1]),
                        reads=[B_scores[i], B_sm], writes=[B_junk2, B_sm])
                    yield L / 960.0 + 0.15
                P.add("dve", lambda h, stepk=stepk: h.scalar_tensor_tensor(
                    out=SM[:, c_dd + T0:c_dd + T1], in0=SM[:, c_cnt + T0:c_cnt + T1], scalar=TOPK - 0.5, in1=stepk,
                    op0=ALU.is_gt, op1=ALU.mult), reads=[B_sm], writes=[B_sm])
                P.add("dve", lambda h: h.tensor_tensor(out=SM[:, c_lo + T0:c_lo + T1], in0=SM[:, c_lo + T0:c_lo + T1],
                                                       in1=SM[:, c_dd + T0:c_dd + T1], op=ALU.add),
                      reads=[B_sm], writes=[B_sm])
                yield 0.5
            for i in range(T0, T1):
                L = 128 * (i + 1)
                sl = i % 2
                P.add("dve", lambda h, i=i, L=L, sl=sl: h.tensor_scalar(
                    out=nmst[sl][:, 0:L], in0=scores[:, offs[i]:offs[i] + L], scalar1=SM[:, c_lo + i:c_lo + i + 1],
                    scalar2=NMV, op0=ALU.is_lt, op1=ALU.mult), reads=[B_scores[i], B_sm], writes=[B_nmst[sl]])
                P.add("sp", lambda h, i=i, L=L, sl=sl: h.dma_start(out=nm_s[i, :, 0:L], in_=nmst[sl][:, 0:L]),
                      reads=[B_nmst[sl]], writes=[B_nmscr], chan=ch_nmst[sl])
                yield L / 960.0 + 0.15

        def merge_threads(gens):
            t = [0.0] * len(gens)
            live = list(range(len(gens)))
            while live:
                k = min(live, key=lambda a: t[a])
                try:
                    t[k] += next(gens[k])
                except StopIteration:
                    live.remove(k)

        ch_odump = new_chan("ch_odump", exact=False)
        e_ctr = [0]
        nmc_ctr = [0]

        def attention_job(subs, is_b, o_ap, B_o, dump_idx, bs, Ebuf, B_E, rden, B_rden, NMc=None, B_NMc=None, ch_NMc=None):
            JQ, JK, JV, JS, B_J, ch_J = bs
            for gi, (qt, kt, vs, vc, row, win) in enumerate(subs):
                P.add("sp", lambda h, gi=gi, qt=qt: h.dma_start(out=JQ[gi], in_=qk_s[qt]), writes=[B_J[gi]], chan=ch_J[gi])
                P.add("sp", lambda h, gi=gi, kt=kt: h.dma_start(out=JK[gi], in_=qk_s[kt]), writes=[B_J[gi]], chan=ch_J[gi])
                vsrc = v_s[vs, :, vc:vc + 128].rearrange("(a p) c -> p a c", p=128)
                P.add("sp", lambda h, gi=gi, vsrc=vsrc: h.dma_start(out=JV[gi], in_=vsrc), writes=[B_J[gi]], chan=ch_J[gi])
                ssrc = bass.AP(tensor=r_t, offset=row * MROW, ap=[[1, 128], [1, WSTRIP]])
                P.add("sp", lambda h, gi=gi, ssrc=ssrc: h.dma_start(out=JS[gi], in_=ssrc), writes=[B_J[gi]], chan=ch_J[gi])
            pending = [None]

            def chunk_gen(c):
                pnum = 3 + (c % 2)
                pden = 5 + (c % 2)
                ns = 0
                if is_b:
                    ns = nmc_ctr[0] % 2
                    nmc_ctr[0] += 1
                    nsrc = nm_s[4 * c:4 * c + 4].rearrange("a p s -> p a s")
                    P.add("sp", lambda h, ns=ns, nsrc=nsrc: h.dma_start(out=NMc[ns], in_=nsrc), reads=[B_nmscr],
                          writes=[B_NMc[ns]], chan=ch_NMc[ns])
                units = []
                for gi, (qt, kt, vs, vc, row, win) in enumerate(subs):
                    jlo = max(0, -((-(512 * c - 127 - win)) // 128))
                    for j in range(jlo, 4 * c + 4):
                        units.append((gi, j))

                def qk(u):
                    gi, j = units[u]
                    pb = u % 3
                    xoff = 512 * c - 128 * j + 384
                    tl = [il for il in range(4) if 4 * c + il >= max(j, 2)] if is_b else []

                    def f(h):
                        h.matmul(ps[pb][:, :], lhsT=JK[gi][:, j * 128:(j + 1) * 128],
                                 rhs=JQ[gi][:, c * 512:(c + 1) * 512], start=True, stop=False)
                        ins = h.matmul(ps[pb][:, :], lhsT=antiJ[:], rhs=JS[gi][:, xoff:xoff + 512], start=False,
                                       stop=(len(tl) == 0))
                        for n_, il in enumerate(tl):
                            ins = h.matmul(ps[pb][:, il * 128:(il + 1) * 128],
                                           lhsT=NMc[ns][:, il, j * 128:(j + 1) * 128], rhs=identB[:],
                                           start=False, stop=(n_ == len(tl) - 1))
                        return ins
                    rd = [B_J[gi], B_const] + ([B_NMc[ns]] if is_b else [])
                    P.add("pe", f, reads=rd, writes=[PS[pb]])

                def ex(u):
                    pb = u % 3
                    eb = e_ctr[0] % 3
                    e_ctr[0] += 1
                    P.add("act", lambda h: h.activation(out=Ebuf[eb], in_=ps[pb][:, :], func=AF.Exp, scale=SCALE),
                          reads=[PS[pb]], writes=[B_E[eb]])
                    return eb

                def pv(u, eb):
                    gi, j = units[u]
                    first = (u == 0)
                    last = (u == len(units) - 1)

                    def f(h):
                        h.matmul(ps[pnum][:, :], lhsT=JV[gi][:, j, :], rhs=Ebuf[eb], start=first, stop=last)
                        return h.matmul(ps[pden][:, :], lhsT=onesB[:], rhs=Ebuf[eb], start=first, stop=last)
                    P.add("pe", f, reads=[B_J[gi], B_E[eb], B_const], writes=[PS[pnum], PS[pden]])

                def norm():
                    P.add("dve", lambda h: h.reciprocal(out=rden, in_=ps[pden][:, :]), reads=[PS[pden]], writes=[B_rden])
                    P.add("dve", lambda h: h.tensor_tensor(out=o_ap[:, c * 512:(c + 1) * 512], in0=ps[pnum][:, :], in1=rden,
                                                           op=ALU.mult), reads=[PS[pnum], B_rden], writes=[B_o])

                qk(0)
                for u in range(len(units)):
                    if u + 1 < len(units):
                        qk(u + 1)
                    eb = ex(u)
                    pv(u, eb)
                    if u == 2 and pending[0] is not None:
                        pending[0]()
                        pending[0] = None
                    yield 1.4 if is_b else 1.2
                pending[0] = norm

            for c in range(4):
                yield from chunk_gen(c)
            pending[0]()
            if debug:
                P.add("sp", lambda h: h.dma_start(out=o_s[dump_idx], in_=o_ap), reads=[B_o], writes=[Buf("dump")],
                      chan=ch_odump)

        if phase_limit >= 4:
            bsA = ([carve(88 + 4 * i, [S], BF16) for i in range(3)], [carve(100 + 4 * i, [S], BF16) for i in range(3)],
                   [carve(112 + 4 * i, [NT, 128], BF16) for i in range(3)], [carve(124 + 5 * i, [WSTRIP], BF16) for i in range(3)],
                   [Buf(f"JA{i}") for i in range(3)], [new_chan(f"ch_JA{i}") for i in range(3)])
            EbufA = [carve(0 + i, [512], BF16) for i in range(3)]
            rdenA = carve(4, [512], F32)
            B_EA = [Buf(f"EA{i}") for i in range(3)]
            B_rdenA = Buf("rdenA")

            def a_thread():
                for hh in range(4):
                    subs = [(FT_AQ + 4 * g + hh, FT_AK + 4 * g + hh, g, hh * 128, 4 * g + hh, WINS[g]) for g in range(3)]
                    yield from attention_job(subs, False, OA[hh], B_OA[hh], hh, bsA, EbufA, B_EA, rdenA, B_rdenA)

            merge_threads([a_thread(), bisect_gen()])
            P.barrier()
            WPA = carve(76, [4, D], BF16)
            WPB = carve(92, [8, D], BF16)
            B_WP = Buf("WP")
            ch_WP = new_chan("ch_WP", exact=False)
            P.add("pool", lambda h: h.dma_start(out=WPA, in_=wpa_d.rearrange("(a p) n -> p a n", p=128)), writes=[B_WP],
                  chan=ch_WP, nobar="out")
            for hf in range(2):
                P.add("pool", lambda h, hf=hf: h.dma_start(out=WPB[:, 4 * hf:4 * hf + 4, :],
                                                           in_=wpb_d[512 * hf:512 * hf + 512, :].rearrange("(a p) n -> p a n", p=128)),
                      writes=[B_WP], chan=ch_WP, nobar="out")
            bsB = []
            for k in range(2):
                o = 20 * k
                bsB.append(([carve(o, [S], BF16)], [carve(o + 4, [S], BF16)], [carve(o + 8, [NT, 128], BF16)],
                            [carve(o + 12, [WSTRIP], BF16)], [Buf(f"JB{k}")], [new_chan(f"ch_JB{k}")]))
            NMc = [carve(37 + 16 * i, [4, S], BF16) for i in range(2)]
            B_NMc = [Buf(f"NMc{i}") for i in range(2)]
            ch_NMc = [new_chan(f"ch_NMc{i}") for i in range(2)]
            EbufB = [carve(69 + i, [512], BF16) for i in range(3)]
            rdenB = carve(72, [512], F32)
            B_EB = [Buf(f"EB{i}") for i in range(3)]
            B_rdenB = Buf("rdenB")

            def b_thread():
                for hb_ in range(8):
                    subs = [(FT_BQ + hb_, FT_BK + hb_, 3 + hb_ // 4, (hb_ % 4) * 128, 12 + hb_, 1 << 30)]
                    yield from attention_job(subs, True, OB[hb_], B_OB[hb_], 4 + hb_, bsB[hb_ % 2], EbufB, B_EB, rdenB,
                                             B_rdenB, NMc, B_NMc, ch_NMc)

            merge_threads([b_thread()])
        P.barrier()

        mergedT = carve(0, [KC, S], BF16)
        B_mg = [Buf(f"mg{i}") for i in range(KC)]
        if phase_limit >= 5:
            sga2 = [carve(124, [S], BF16), carve(64, [S], BF16)]
            sgb2 = [carve(128, [S], BF16), carve(68, [S], BF16)]
            tmpa = [carve(132 + 2 * i, [512], F32) for i in range(2)]
            tmpb = [carve(136 + 2 * i, [512], F32) for i in range(2)]
            B_sga2, B_sgb2 = [Buf("sga0"), Buf("sga1")], [Buf("sgb0"), Buf("sgb1")]
            ch_sga2 = [new_chan("ch_sga0"), new_chan("ch_sga1")]
            ch_sgb2 = [new_chan("ch_sgb0"), new_chan("ch_sgb1")]
            B_tmpa = [Buf(f"tmpa{i}") for i in range(2)]
            B_tmpb = [Buf(f"tmpb{i}") for i in range(2)]
            u_ctr = 0
            for dt in range(KC):
                sga, sgb = sga2[dt % 2], sgb2[dt % 2]
                B_sga, B_sgb = B_sga2[dt % 2], B_sgb2[dt % 2]
                P.add("sp", lambda h, dt=dt, sga=sga: h.dma_start(out=sga, in_=gate_s[dt]), writes=[B_sga],
                      chan=ch_sga2[dt % 2])
                P.add("sp", lambda h, dt=dt, sgb=sgb: h.dma_start(out=sgb, in_=gate_s[16 + dt]), writes=[B_sgb],
                      chan=ch_sgb2[dt % 2])
                for tc in range(4):
                    pa = 2 * (u_ctr % 2)
                    pbk = pa + 1
                    tsl = u_ctr % 2
                    u_ctr += 1
                    def mma(h, dt=dt, tc=tc, pa=pa):
                        for a in range(4):
                            ins = h.matmul(ps[pa][:, :], lhsT=WPA[:, a, dt * 128:(dt + 1) * 128],
                                           rhs=OA[a][:, tc * 512:(tc + 1) * 512], start=(a == 0), stop=(a == 3))
                        return ins
                    P.add("pe", mma, reads=[B_WP] + B_OA, writes=[PS[pa]])
                    def mmb(h, dt=dt, tc=tc, pbk=pbk):
                        for a in range(8):
                            ins = h.matmul(ps[pbk][:, :], lhsT=WPB[:, a, dt * 128:(dt + 1) * 128],
                                           rhs=OB[a][:, tc * 512:(tc + 1) * 512], start=(a == 0), stop=(a == 7))
                        return ins
                    P.add("pe", mmb, reads=[B_WP] + B_OB, writes=[PS[pbk]])
                    P.add("dve", lambda h, pa=pa, tc=tc, tsl=tsl, sga=sga: h.tensor_tensor(
                        out=tmpa[tsl], in0=ps[pa][:, :], in1=sga[:, tc * 512:(tc + 1) * 512], op=ALU.mult),
                        reads=[PS[pa], B_sga], writes=[B_tmpa[tsl]])
                    P.add("dve", lambda h, pbk=pbk, tc=tc, tsl=tsl, sgb=sgb: h.tensor_tensor(
                        out=tmpb[tsl], in0=ps[pbk][:, :], in1=sgb[:, tc * 512:(tc + 1) * 512], op=ALU.mult),
                        reads=[PS[pbk], B_sgb], writes=[B_tmpb[tsl]])
                    P.add("pool", lambda h, dt=dt, tc=tc, tsl=tsl: h.tensor_tensor(
                        out=mergedT[:, dt, tc * 512:(tc + 1) * 512], in0=tmpa[tsl], in1=tmpb[tsl], op=ALU.add),
                        reads=[B_tmpa[tsl], B_tmpb[tsl]], writes=[B_mg[dt]])
        P.barrier()

        B_x1scr = Buf("x1scr")
        if phase_limit >= 6:
            wsl5 = [carve(72 + 16 * i, [16, 512], BF16) for i in range(3)]
            xin = [carve(120 + 2 * i, [512], F32) for i in range(4)]
            x1p = [carve(128 + 2 * i, [512], F32) for i in range(4)]
            junk5 = carve(136, [512], BF16)
            B_xin = [Buf(f"xin{i}") for i in range(4)]
            ch_xin = [new_chan(f"ch_xin{i}") for i in range(4)]
            B_x1p = [Buf(f"x1p{i}") for i in range(4)]
            ch_x1p = [new_chan(f"ch_x1p{i}") for i in range(4)]
            B_junk5 = Buf("junk5")
            wout_v = wout_d.rearrange("(k p) n -> p k n", p=128)
            def xin_load(uu):
                dc_, i_ = divmod(uu, NT)
                sl_ = uu % 4
                P.add("sp", lambda h: h.dma_start(out=xin[sl_], in_=x_d[i_ * 128:(i_ + 1) * 128, dc_ * 512:(dc_ + 1) * 512]),
                      writes=[B_xin[sl_]], chan=ch_xin[sl_])

            xin_load(0)
            xin_load(1)
            u = 0
            for dc in range(4):
                s = load_slab(wout_v[:, :, dc * 512:(dc + 1) * 512], wsl5)
                W = wsl5[s]
                for i in range(NT):
                    sl = u % 4
                    if u + 2 < 4 * NT:
                        xin_load(u + 2)
                    u += 1
                    pb = next_ps()
                    def mm(h, i=i, W=W, pb=pb):
                        for kc in range(KC):
                            ins = h.matmul(ps[pb][:, :], lhsT=mergedT[:, kc, i * 128:(i + 1) * 128], rhs=W[:, kc, :],
                                           start=(kc == 0), stop=(kc == KC - 1))
                        return ins
                    P.add("pe", mm, reads=[B_wsl[s]] + B_mg, writes=[PS[pb]])
                    P.add("dve", lambda h, pb=pb, sl=sl: h.tensor_tensor(out=x1p[sl], in0=ps[pb][:, :], in1=xin[sl],
                                                                         op=ALU.add),
                          reads=[PS[pb], B_xin[sl]], writes=[B_x1p[sl]])
                    P.add("act", lambda h, i=i, dc=dc, sl=sl: h.activation(
                        out=junk5, in_=x1p[sl], func=AF.Square, accum_out=SM[:, c_ssq1 + 4 * i + dc:c_ssq1 + 4 * i + dc + 1]),
                        reads=[B_x1p[sl]], writes=[B_junk5, B_sm])
                    P.add("sp", lambda h, i=i, dc=dc, sl=sl: h.dma_start(
                        out=x1_s[i * 128:(i + 1) * 128, dc * 512:(dc + 1) * 512], in_=x1p[sl]), reads=[B_x1p[sl]],
                        writes=[B_x1scr], chan=ch_x1p[sl])
        P.barrier()

        ch_out = new_chan("ch_out", exact=False)
        P.out_chans.append(ch_out)
        if phase_limit >= 7:
            hmT2 = [carve(0, [KC, 512], BF16), carve(56, [KC, 512], BF16)]
            actT = carve(120, [64, 512], BF16)
            wsl6 = [carve(72 + 16 * i, [16, 512], BF16) for i in range(3)]
            junk6_ap = carve(188, [512], BF16)
            x1t = [carve(16 + 8 * i, [D], F32) for i in range(4)]
            gfin = carve(48, [D], F32)
            hb6 = carve(190, [D], F32)
            sq = [carve(184 + 2 * i, [512], F32) for i in range(2)]
            B_hmT2, B_gf, B_hb6 = [Buf("hmT0"), Buf("hmT1")], Buf("gfin"), Buf("hb6")
            ch_hb6 = new_chan("ch_hb6")
            B_actT = [Buf(f"actT{i}") for i in range(64)]
            B_x1t = [Buf(f"x1t{i}") for i in range(4)]
            ch_x1t = [new_chan(f"ch_x1t{i}") for i in range(4)]
            B_sq = [Buf(f"sq{i}") for i in range(2)]
            B_junk6 = Buf("junk6")
            B_gT = Buf("gT")
            wup_v = wupb_s.rearrange("(k p) n -> p k n", p=128)
            wdn_v = wdnb_s.rearrange("(k p) n -> p k n", p=128)
            P.add("sp", lambda h: h.dma_start(out=gfin, in_=gfin_d.partition_broadcast(128)), writes=[B_gf], chan=ch_misc)
            g16 = carve(199, [128], F32)
            B_g16 = Buf("g16")
            P.add("sp", lambda h: h.dma_start(out=g16[0:16, :], in_=gmlp_d.rearrange("(k p) -> k p", p=128)),
                  writes=[B_g16], chan=ch_misc)
            P.add("pe", lambda h: h.transpose(out=ps[7][:, 0:16], in_=g16[0:16, :], identity=identF[0:16, 0:16]),
                  reads=[B_g16, B_const], writes=[PS[7]])
            P.add("dve", lambda h: h.tensor_copy(out=SM[:, c_gT:c_gT + KC], in_=ps[7][:, 0:16]), reads=[PS[7]],
                  writes=[B_gT])
            ssq1_v = SM[:, c_ssq1:c_ssq1 + 64].rearrange("p (a b) -> p a b", b=4)
            P.add("dve", lambda h: h.tensor_reduce(out=SM[:, c_t1:c_t1 + 16], in_=ssq1_v, axis=AX.X, op=ALU.add),
                  reads=[B_sm], writes=[B_sm])
            rstd_ops(c_t1, 16, c_rt, c_rstd, 1.0 / D)
            prep_ps = [0]

            def prep_a(tc_, il):
                i = 4 * tc_ + il
                P.add("sp", lambda h: h.dma_start(out=hb6, in_=x1_s[i * 128:(i + 1) * 128, :]), reads=[B_x1scr],
                      writes=[B_hb6], chan=ch_hb6)
                P.add("act", lambda h: h.activation(out=hb6, in_=hb6, func=AF.Identity,
                                                    scale=SM[:, c_rstd + i:c_rstd + i + 1]),
                      reads=[B_hb6, B_sm], writes=[B_hb6])

            def prep_b(tc_, il):
                hm = hmT2[tc_ % 2]
                for q in range(4):
                    pb = 4 + prep_ps[0] % 4
                    prep_ps[0] += 1
                    def tr(h, q=q, pb=pb):
                        for j in range(4):
                            kc = 4 * q + j
                            ins = h.transpose(out=ps[pb][:, j * 128:(j + 1) * 128], in_=hb6[:, kc * 128:(kc + 1) * 128],
                                              identity=identF[:])
                        return ins
                    P.add("pe", tr, reads=[B_hb6, B_const], writes=[PS[pb]])
                    for j in range(4):
                        kc = 4 * q + j
                        evac_copy(hm[:, kc, il * 128:(il + 1) * 128], ps[pb][:, j * 128:(j + 1) * 128], [PS[pb], B_gT],
                                  [B_hmT2[tc_ % 2]], scale=SM[:, c_gT + kc:c_gT + kc + 1])

            def x1t_load(tc_, il):
                i = 4 * tc_ + il
                P.add("sp", lambda h: h.dma_start(out=x1t[il], in_=x1_s[i * 128:(i + 1) * 128, :]),
                      reads=[B_x1scr], writes=[B_x1t[il]], chan=ch_x1t[il])

            for il in range(4):
                prep_a(0, il)
                prep_b(0, il)
            uq = 0
            for tcc in range(4):
                hmT = hmT2[tcc % 2]
                B_hmT = B_hmT2[tcc % 2]
                for il in range(4):
                    x1t_load(tcc, il)
                for su in range(16):
                    s = load_slab(wup_v[:, :, su * 512:(su + 1) * 512], wsl6, reads=B_cv, nobar=("in" if (tcc == 0 and su < 3) else False))
                    W = wsl6[s]
                    for ft in range(4):
                        f_idx = 4 * su + ft
                        if tcc + 1 < 4:
                            if f_idx % 16 == 0:
                                prep_a(tcc + 1, f_idx // 16)
                            elif f_idx % 16 == 8:
                                prep_b(tcc + 1, f_idx // 16)
                        pb = next_ps()
                        qs = uq % 2
                        uq += 1
                        def mm(h, ft=ft, W=W, pb=pb, hmT=hmT):
                            for kc in range(KC):
                                ins = h.matmul(ps[pb][:, :], lhsT=W[:, kc, ft * 128:(ft + 1) * 128], rhs=hmT[:, kc, :],
                                               start=(kc == 0), stop=(kc == KC - 1))
                            return ins
                        P.add("pe", mm, reads=[B_wsl[s], B_hmT], writes=[PS[pb]])
                        P.add("act", lambda h, pb=pb, qs=qs: h.activation(out=sq[qs], in_=ps[pb][:, :], func=AF.Square),
                              reads=[PS[pb]], writes=[B_sq[qs]])
                        P.add("dve", lambda h, pb=pb, qs=qs, f_idx=f_idx: h.scalar_tensor_tensor(
                            out=actT[:, f_idx, :], in0=ps[pb][:, :], scalar=0.0, in1=sq[qs], op0=ALU.is_gt, op1=ALU.mult),
                            reads=[PS[pb], B_sq[qs]], writes=[B_actT[f_idx]])
                for dc in range(4):
                    pbase = 4 * (dc % 2)
                    for sd in range(4):
                        s = load_slab(wdn_v[:, 16 * sd:16 * sd + 16, dc * 512:(dc + 1) * 512], wsl6, reads=B_cv)
                        W = wsl6[s]
                        def mm(h, sd=sd, W=W, pbase=pbase):
                            for fl in range(16):
                                fc = 16 * sd + fl
                                for il in range(4):
                                    ins = h.matmul(ps[pbase + il][:, :], lhsT=actT[:, fc, il * 128:(il + 1) * 128],
                                                   rhs=W[:, fl, :], start=(fc == 0), stop=(fc == 63))
                            return ins
                        P.add("pe", mm, reads=[B_wsl[s]] + B_actT[16 * sd:16 * sd + 16], writes=PS[pbase:pbase + 4])
                    for il in range(4):
                        i = 4 * tcc + il
                        P.add("dve", lambda h, il=il, dc=dc, pbase=pbase: h.tensor_tensor(
                            out=x1t[il][:, dc * 512:(dc + 1) * 512], in0=ps[pbase + il][:, :],
                            in1=x1t[il][:, dc * 512:(dc + 1) * 512], op=ALU.add),
                            reads=[PS[pbase + il], B_x1t[il]], writes=[B_x1t[il]])
                        P.add("act", lambda h, il=il, dc=dc, i=i: h.activation(
                            out=junk6_ap, in_=x1t[il][:, dc * 512:(dc + 1) * 512], func=AF.Square,
                            accum_out=SM[:, c_ssq2 + 4 * i + dc:c_ssq2 + 4 * i + dc + 1]),
                            reads=[B_x1t[il]], writes=[B_junk6, B_sm])
                ssq2_v = SM[:, c_ssq2 + 16 * tcc:c_ssq2 + 16 * tcc + 16].rearrange("p (a b) -> p a b", b=4)
                P.add("dve", lambda h, tcc=tcc, ssq2_v=ssq2_v: h.tensor_reduce(
                    out=SM[:, c_t2 + 4 * tcc:c_t2 + 4 * tcc + 4], in_=ssq2_v, axis=AX.X, op=ALU.add),
                    reads=[B_sm], writes=[B_sm])
                rstd_ops(c_t2 + 4 * tcc, 4, c_s1 + 4 * tcc, c_s2 + 4 * tcc, 1.0 / D)
                for il in range(4):
                    i = 4 * tcc + il
                    P.add("dve", lambda h, i=i, il=il: h.scalar_tensor_tensor(
                        out=x1t[il], in0=x1t[il], scalar=SM[:, c_s2 + i:c_s2 + i + 1], in1=gfin, op0=ALU.mult,
                        op1=ALU.mult), reads=[B_x1t[il], B_sm, B_gf], writes=[B_x1t[il]])
                    P.add("sp", lambda h, i=i, il=il: h.dma_start(out=out_d[i * 128:(i + 1) * 128, :], in_=x1t[il]),
                          reads=[B_x1t[il]], writes=[Buf("out")], chan=ch_out)
        else:
            z = carve(0, [D], F32)
            Bz = Buf("z")
            P.add("pool", lambda h: h.memset(z, 0.0), writes=[Bz])
            P.add("sp", lambda h: h.dma_start(out=out_d[0:128, :], in_=z), reads=[Bz], writes=[Buf("out")], chan=ch_out)

        P.emit(block, engsem)
    return nc


_CACHE = {}


def _host_consts():
    if "c" not in _CACHE:
        _CACHE["c"] = (_onehot_tables(), (2.0 ** -(np.arange(NIT) + 1)).astype(np.float32))
    return _CACHE["c"]


def make_in_map(inputs, b):
    oh, pw2 = _host_consts()
    f = lambda a: np.ascontiguousarray(np.asarray(a, dtype=np.float32))
    return {
        "x": f(inputs["x"][b]),
        "w_in": f(inputs["w_in"][0]),
        "w_proj_a": f(inputs["w_proj_a"][0]),
        "w_proj_b": f(inputs["w_proj_b"][0]),
        "w_out": f(inputs["w_out"][0]),
        "w_mlp_up": f(inputs["w_mlp_up"][0]),
        "w_mlp_down": f(inputs["w_mlp_down"][0]),
        "norm_mix_g": f(inputs["norm_mix_g"][0]),
        "norm_mlp_g": f(inputs["norm_mlp_g"][0]),
        "norm_final_g": f(inputs["norm_final_g"]),
        "idx_k_norm_g": f(inputs["idx_k_norm_g"][0]),
        "idx_k_norm_b": f(inputs["idx_k_norm_b"][0]),
        "rel_bias_table": f(inputs["rel_bias_table"]),
        "onehot": oh,
        "pow2": pw2,
    }


def kernel(**inputs):
    nc = build_program()
    n = 8
    in_maps = [make_in_map(inputs, b) for b in range(n)]
    res = run_bass_kernel_spmd(nc, in_maps, core_ids=list(range(n)))
    return np.stack([np.asarray(r["out"], dtype=np.float32) for r in res.results], axis=0)
```

```python
import bisect
from contextlib import ExitStack

import numpy as np
import concourse.bass as bass
import concourse.mybir as mybir
from concourse.bass_utils import run_bass_kernel_spmd

F32 = mybir.dt.float32
BF16 = mybir.dt.bfloat16
AF = mybir.ActivationFunctionType
ALU = mybir.AluOpType
AX = mybir.AxisListType

S = 2048
D = 2048
DIN = 12880
DFF = 8192
NT = 16
KC = 16
EPS = 1e-6
SCALE = 128.0 ** -0.5
TOPK = 256
NIT = 14
NEG_RAW = -3000.0
NEGV = NEG_RAW / SCALE
NMV = -30000.0
MROW = 2559
WSTRIP = 2432
DILS = (1, 4, 16)
WINS = (128, 512, 2048)

C_QKVA = 0
C_QKVB = 4608
C_QIDX = 7680
C_KIDX = 8704
C_WIDX = 8768
C_GA = 8784
C_GB = 10832

FT_AQ = 0
FT_AK = 12
FT_BQ = 24
FT_BK = 32
FT_QI = 40
NFT = 48


class Buf:
    __slots__ = ("name", "wr", "rd")

    def __init__(self, name=""):
        self.name = name
        self.wr = {}
        self.rd = {}


class Chan:
    def __init__(self, sem, exact=True):
        self.sem = sem
        self.exact = exact
        self.ids = []


class Op:
    __slots__ = ("id", "eng", "fn", "deps", "chan", "ord", "inc", "incval")


def _ids(d):
    out = set()
    for k, v in d.items():
        if k == "dma":
            out.update(v)
        else:
            out.add(v)
    return out


class Prog:
    ENGS = ("pe", "act", "dve", "pool", "sp")

    def __init__(self):
        self.ops = []
        self.bar = set()
        self.bar_pending = {e: False for e in self.ENGS}
        self.last = {}
        self.dma_since = []
        self.out_chans = []

    def _addto(self, d, op):
        if op.chan is None:
            d[op.eng] = op.id
        else:
            d.setdefault("dma", []).append(op.id)

    def add(self, eng, fn, reads=(), writes=(), chan=None, nobar=False):
        op = Op()
        op.id = len(self.ops)
        op.eng = eng
        op.fn = fn
        op.chan = chan
        op.inc = False
        op.incval = 0
        op.ord = 0
        raw = set()
        oth = set()
        for b in reads:
            raw |= _ids(b.wr)
        for b in writes:
            oth |= _ids(b.wr)
            oth |= _ids(b.rd)
        if self.bar_pending[eng] and nobar not in (True, "in"):
            oth |= self.bar
            self.bar_pending[eng] = False
        final = set()
        for d in raw | oth:
            o = self.ops[d]
            if o.chan is not None or o.eng != eng:
                final.add(d)
            elif eng in ("act", "dve", "pool") and d in raw:
                final.add(d)
        op.deps = final
        for b in reads:
            if b not in writes:
                self._addto(b.rd, op)
        for b in writes:
            if b.rd:
                b.wr = {}
                b.rd = {}
            self._addto(b.wr, op)
        if chan is not None:
            chan.ids.append(op.id)
            op.ord = len(chan.ids)
            if nobar not in (True, "out"):
                self.dma_since.append(op.id)
        if nobar not in (True, "out"):
            self.last[eng] = op.id
        self.ops.append(op)
        return op

    def barrier(self):
        self.bar = set(self.last.values()) | set(self.dma_since)
        self.dma_since = []
        for e in self.ENGS:
            self.bar_pending[e] = True

    def emit(self, block, engsem):
        ops = self.ops
        for op in ops:
            for d in op.deps:
                if ops[d].chan is None:
                    ops[d].inc = True
        cnt = {e: 0 for e in self.ENGS}
        for op in ops:
            if op.chan is None and op.inc:
                cnt[op.eng] += 1
                op.incval = cnt[op.eng]
        by_eng = {e: [op for op in ops if op.eng == e] for e in self.ENGS}
        out_chans = self.out_chans

        def run(h, eng):
            waited = {}
            for op in by_eng[eng]:
                need = {}
                for d in op.deps:
                    o = ops[d]
                    if o.chan is not None:
                        key = ("c", id(o.chan))
                        sem = o.chan.sem
                        if o.chan.exact:
                            val = 16 * o.ord
                        else:
                            val = 16 * bisect.bisect_left(o.chan.ids, op.id)
                    else:
                        key = ("e", o.eng)
                        sem = engsem[o.eng]
                        val = o.incval
                    if key not in need or need[key][1] < val:
                        need[key] = (sem, val)
                for key, (sem, val) in need.items():
                    if waited.get(key, 0) < val:
                        h.wait_ge(sem, val)
                        waited[key] = val
                ins = op.fn(h)
                if op.chan is not None:
                    ins.then_inc(op.chan.sem, 16)
                elif op.inc:
                    ins.then_inc(engsem[eng], 1)
            if eng == "sp":
                for ch in out_chans:
                    h.wait_ge(ch.sem, 16 * len(ch.ids))

        @block.tensor
        def _(h):
            run(h, "pe")

        @block.scalar
        def _(h):
            run(h, "act")

        @block.vector
        def _(h):
            run(h, "dve")

        @block.gpsimd
        def _(h):
            run(h, "pool")

        @block.sync
        def _(h):
            run(h, "sp")


def _t5_bucket_np(n):
    n = np.maximum(n, 0)
    nf = np.maximum(n, 1).astype(np.float32)
    large = 16 + (np.log(nf / np.float32(16)) / np.float32(np.log(2048 / 16)) * np.float32(16)).astype(np.int32)
    large = np.minimum(large, 31)
    return np.where(n < 16, n, large)


def _onehot_tables():
    m = np.arange(MROW)
    n = m - 511
    bk = _t5_bucket_np(n)
    oh = np.zeros((4, 33, MROW), np.float32)
    for v in range(4):
        if v < 3:
            dil, win = DILS[v], WINS[v]
            valid = (n >= 0) & (n % dil == 0) & (n <= win)
        else:
            valid = n >= 0
        oh[v, bk[valid], m[valid]] = 1.0
        oh[v, 32, m[~valid]] = 1.0
    return oh


def build_program(debug=False, phase_limit=99):
    nc = bass.Bass("TRN2", target_bir_lowering=False)
    dram = lambda name, shape, dt, kind="ExternalInput": nc.dram_tensor(name, shape, dt, kind=kind)
    x_d = dram("x", [S, D], F32).ap()
    w_in_d = dram("w_in", [D, DIN], F32).ap()
    wpa_d = dram("w_proj_a", [512, D], F32).ap()
    wpb_d = dram("w_proj_b", [1024, D], F32).ap()
    wout_d = dram("w_out", [D, D], F32).ap()
    wup_d = dram("w_mlp_up", [D, DFF], F32).ap()
    wdn_d = dram("w_mlp_down", [DFF, D], F32).ap()
    gmix_d = dram("norm_mix_g", [D], F32).ap()
    gmlp_d = dram("norm_mlp_g", [D], F32).ap()
    gfin_d = dram("norm_final_g", [D], F32).ap()
    ikg_d = dram("idx_k_norm_g", [64], F32).ap()
    ikb_d = dram("idx_k_norm_b", [64], F32).ap()
    tab_d = dram("rel_bias_table", [32, 20], F32).ap()
    oh_d = dram("onehot", [4, 33, MROW], F32).ap()
    pw2_d = dram("pow2", [NIT], F32).ap()
    out_d = dram("out", [S, D], F32, kind="ExternalOutput").ap()
    skind = "ExternalOutput" if debug else "Internal"
    qk_t = dram("qk_scr", [NFT, 128, S], BF16, kind=skind)
    v_t = dram("v_scr", [5, S, 512], BF16, kind=skind)
    gate_t = dram("gate_scr", [32, 128, S], BF16, kind=skind)
    r_t = dram("r_scr", [20, MROW], BF16, kind=skind)
    nm_t = dram("nm_scr", [NT, 128, S], BF16, kind=skind)
    x1_t = dram("x1_scr", [S, D], F32, kind=skind)
    ob_t = dram("o_scr", [12, 128, S], BF16, kind=skind)
    wupb_t = dram("wupb_scr", [D, DFF], BF16, kind="Internal")
    wdnb_t = dram("wdnb_scr", [DFF, D], BF16, kind="Internal")
    wupb_s, wdnb_s = wupb_t.ap(), wdnb_t.ap()
    qk_s, v_s, gate_s, r_s, nm_s, x1_s, o_s = (t.ap() for t in (qk_t, v_t, gate_t, r_t, nm_t, x1_t, ob_t))

    P = Prog()
    es = ExitStack()
    with es:
        ARENA_W = 200 * 256
        arena = es.enter_context(nc.sbuf_tensor("arena", [128, ARENA_W], F32))
        identF = es.enter_context(nc.sbuf_tensor("identF", [128, 128], F32))
        identB = es.enter_context(nc.sbuf_tensor("identB", [128, 128], BF16))
        antiJ = es.enter_context(nc.sbuf_tensor("antiJ", [128, 128], BF16))
        onesB = es.enter_context(nc.sbuf_tensor("onesB", [128, 128], BF16))
        onesF = es.enter_context(nc.sbuf_tensor("onesF", [128, 128], F32))
        SM = es.enter_context(nc.sbuf_tensor("SM", [128, 1024], F32))
        ps = [es.enter_context(nc.psum_tensor(f"ps{i}", [128, 512], F32)) for i in range(8)]
        engsem = {e: es.enter_context(nc.semaphore(f"sem_{e}")) for e in ("pe", "act", "dve", "pool")}

        def new_chan(name, exact=True):
            return Chan(es.enter_context(nc.semaphore(name)), exact)

        block = es.enter_context(nc.Block())

        PS = [Buf(f"ps{i}") for i in range(8)]
        B_const = Buf("const")
        ch_misc = new_chan("ch_misc", exact=False)

        def carve(off_kb, shape, dt):
            n = int(np.prod(shape))
            esz = 4 if dt == F32 else 2
            nbytes = n * esz
            w0 = int(off_kb * 256)
            nw = (nbytes + 3) // 4
            assert w0 + nw <= ARENA_W, (off_kb, shape)
            a = arena[:, w0:w0 + nw]
            if dt != F32:
                a = a.bitcast(dt)[:, 0:n]
            if len(shape) == 2:
                a = a.rearrange("p (a b) -> p a b", a=shape[0])
            elif len(shape) == 3:
                a = a.rearrange("p (a b c) -> p a b c", a=shape[0], b=shape[1])
            return a

        c_ssq = 0
        c_rt = 16
        c_rstd = 32
        c_eps = 48
        c_ikg = 50
        c_ikb = 51
        c_s1 = 64
        c_negmu = 80
        c_s2 = 96
        c_sd = 112
        c_rs = 128
        c_lo = 144
        c_w0 = 160
        c_mid = 176
        c_cnt = 192
        c_dd = 208
        c_ssq1 = 224
        c_ssq2 = 288
        c_t1 = 352
        c_t2 = 368
        c_pw2 = 384
        c_steps = 416
        c_hi = 800
        c_gT = 820

        def mk_consts():
            for t_, pat, base in ((identF, [[-1, 128]], 0), (identB, [[-1, 128]], 0), (antiJ, [[1, 128]], -127)):
                bt = Buf("c_tmp")
                P.add("pool", lambda h, t_=t_: h.memset(t_[:], 1.0), writes=[bt])
                P.add("pool", lambda h, t_=t_, pat=pat, base=base: h.affine_select(
                    out=t_[:], in_=t_[:], pattern=pat, compare_op=ALU.is_equal, fill=0.0, base=base, channel_multiplier=1),
                    reads=[bt], writes=[bt, B_const])
            P.add("pool", lambda h: h.memset(onesB[:], 1.0), writes=[B_const])
            P.add("pool", lambda h: h.memset(onesF[:], 1.0), writes=[B_const])
            P.add("pool", lambda h: h.memset(SM[:, c_eps:c_eps + 1], EPS), writes=[B_const])
            P.add("sp", lambda h: h.dma_start(out=SM[:, c_pw2:c_pw2 + NIT], in_=pw2_d.partition_broadcast(128)),
                  writes=[B_const], chan=ch_misc)
            P.add("sp", lambda h: h.dma_start(out=SM[0:64, c_ikg:c_ikg + 1], in_=ikg_d.rearrange("(p o) -> p o", o=1)),
                  writes=[B_const], chan=ch_misc)
            P.add("sp", lambda h: h.dma_start(out=SM[64:128, c_ikg:c_ikg + 1], in_=ikg_d.rearrange("(p o) -> p o", o=1)),
                  writes=[B_const], chan=ch_misc)
            P.add("sp", lambda h: h.dma_start(out=SM[0:64, c_ikb:c_ikb + 1], in_=ikb_d.rearrange("(p o) -> p o", o=1)),
                  writes=[B_const], chan=ch_misc)
            P.add("sp", lambda h: h.dma_start(out=SM[64:128, c_ikb:c_ikb + 1], in_=ikb_d.rearrange("(p o) -> p o", o=1)),
                  writes=[B_const], chan=ch_misc)

        mk_consts()

        def mk_rrows():
            tabX = carve(0, [20], F32)
            ohs = carve(1, [MROW], F32)
            rsb = carve(12, [MROW], BF16)
            B_tab, B_oh, B_rsb = Buf("tab"), Buf("oh"), Buf("rsb")
            P.add("pool", lambda h: h.memset(tabX[32:33, :], NEG_RAW), writes=[B_tab])
            P.add("sp", lambda h: h.dma_start(out=tabX[0:32, :], in_=tab_d), writes=[B_tab], chan=ch_misc)
            for v in range(4):
                nh = 4 if v < 3 else 8
                c0 = v * 4
                P.add("sp", lambda h, v=v: h.dma_start(out=ohs[0:33, :], in_=oh_d[v]), writes=[B_oh], chan=ch_misc)
                for k in range(5):
                    w = min(512, MROW - 512 * k)
                    pb = k % 2
                    P.add("pe", lambda h, k=k, w=w, pb=pb, c0=c0, nh=nh: h.matmul(
                        ps[pb][0:nh, 0:w], lhsT=tabX[0:33, c0:c0 + nh], rhs=ohs[0:33, 512 * k:512 * k + w],
                        start=True, stop=True), reads=[B_tab, B_oh, B_const], writes=[PS[pb]])
                    P.add("act", lambda h, k=k, w=w, pb=pb, nh=nh: h.activation(
                        out=rsb[0:nh, 512 * k:512 * k + w], in_=ps[pb][0:nh, 0:w], func=AF.Copy, scale=1.0 / SCALE),
                        reads=[PS[pb]], writes=[B_rsb])
                P.add("sp", lambda h, c0=c0, nh=nh: h.dma_start(out=r_s[c0:c0 + nh, :], in_=rsb[0:nh, :]),
                      reads=[B_rsb], writes=[B_rscr], chan=ch_misc)

        B_rscr = Buf("r_scr")
        mk_rrows()
        P.barrier()

        kw = carve(0, [NT, 80], F32)
        hT = carve(8, [KC, S], BF16)
        wsl = [carve(72 + 16 * i, [16, 512], BF16) for i in range(3)]
        XT = [carve(120 + 8 * i, [D], F32) for i in range(2)]
        hbs = [carve(136, [D], F32), carve(176, [D], F32)]
        junk = carve(144, [D], BF16)
        gbc = carve(148, [D], F32)
        stFM = [carve(156 + 4 * i, [S], BF16) for i in range(3)]
        stTM = [carve(168 + 4 * i, [4, 512], BF16) for i in range(2)]
        B_kw = Buf("kw")
        B_hT = [Buf(f"hT{i}") for i in range(NT)]
        B_wsl = [Buf(f"wsl{i}") for i in range(3)]
        ch_wsl = [new_chan(f"ch_wsl{i}") for i in range(3)]
        B_XT = [Buf(f"XT{i}") for i in range(2)]
        ch_XT = [new_chan(f"ch_XT{i}") for i in range(2)]
        B_hbs, B_junkA, B_junkD, B_gbc = [Buf("hb0"), Buf("hb1")], Buf("junkA"), Buf("junkD"), Buf("gbc")
        B_stFM = [Buf(f"stFM{i}") for i in range(3)]
        ch_stFM = [new_chan(f"ch_stFM{i}") for i in range(3)]
        B_stTM = [Buf(f"stTM{i}") for i in range(2)]
        ch_stTM = [new_chan(f"ch_stTM{i}") for i in range(2)]
        B_sm = Buf("sm")

        evac_rr = [0]

        def evac_copy(out_ap, in_ap, reads, writes, scale=None):
            evac_rr[0] ^= 1
            if evac_rr[0]:
                if scale is None:
                    P.add("act", lambda h: h.activation(out=out_ap, in_=in_ap, func=AF.Copy), reads=reads, writes=writes)
                else:
                    fn_ = AF.Copy if isinstance(scale, float) else AF.Identity
                    P.add("act", lambda h: h.activation(out=out_ap, in_=in_ap, func=fn_, scale=scale),
                          reads=reads, writes=writes)
            else:
                if scale is None:
                    P.add("dve", lambda h: h.tensor_copy(out=out_ap, in_=in_ap), reads=reads, writes=writes)
                else:
                    P.add("dve", lambda h: h.tensor_scalar(out=out_ap, in0=in_ap, scalar1=scale, scalar2=None,
                                                           op0=ALU.mult), reads=reads, writes=writes)

        def rstd_ops(ssq_col, n, rt_col, rstd_col, inv_n, bsm=None):
            bsm = bsm or B_sm
            P.add("act", lambda h: h.activation(out=SM[:, rt_col:rt_col + n], in_=SM[:, ssq_col:ssq_col + n],
                                                func=AF.Sqrt, scale=inv_n, bias=SM[:, c_eps:c_eps + 1]),
                  reads=[bsm, B_const], writes=[bsm])
            P.add("dve", lambda h: h.reciprocal(out=SM[:, rstd_col:rstd_col + n], in_=SM[:, rt_col:rt_col + n]),
                  reads=[bsm], writes=[bsm])

        P.add("sp", lambda h: h.dma_start(out=gbc, in_=gmix_d.partition_broadcast(128)), writes=[B_gbc], chan=ch_misc)
        B_xld = Buf("xld")

        def p1_stage_a(i):
            sl = i % 2
            P.add("sp", lambda h: h.dma_start(out=XT[sl], in_=x_d[i * 128:(i + 1) * 128, :]),
                  writes=[B_XT[sl]] + ([B_xld] if i == 11 else []), chan=ch_XT[sl])
            bsm_i = Buf(f"smP1_{i}")
            hb = hbs[sl]
            P.add("act", lambda h: h.activation(out=junk, in_=XT[sl], func=AF.Square,
                                                accum_out=SM[:, c_ssq + i:c_ssq + i + 1]),
                  reads=[B_XT[sl]], writes=[B_junkA, bsm_i])
            rstd_ops(c_ssq + i, 1, c_rt + i, c_rstd + i, 1.0 / D, bsm=bsm_i)
            P.add("dve", lambda h: h.scalar_tensor_tensor(
                out=hb, in0=XT[sl], scalar=SM[:, c_rstd + i:c_rstd + i + 1], in1=gbc, op0=ALU.mult, op1=ALU.mult),
                reads=[B_XT[sl], bsm_i, B_gbc], writes=[B_hbs[sl]])

        def p1_stage_b(i):
            sl = i % 2
            hb = hbs[sl]
            for q in range(4):
                pb = q % 4
                def tr(h, q=q, pb=pb):
                    for j in range(4):
                        kc = 4 * q + j
                        ins = h.transpose(out=ps[pb][:, j * 128:(j + 1) * 128], in_=hb[:, kc * 128:(kc + 1) * 128],
                                          identity=identF[:])
                    return ins
                P.add("pe", tr, reads=[B_hbs[sl], B_const], writes=[PS[pb]])
                evac_copy(hT[:, 4 * q:4 * q + 4, i * 128:(i + 1) * 128],
                          ps[pb][:, :].rearrange("p (a b) -> p a b", a=4), [PS[pb]], [B_hT[i]])

        p1_stage_a(0)
        for i in range(NT):
            if i + 1 < NT:
                p1_stage_a(i + 1)
            p1_stage_b(i)

        w_in_v = w_in_d.rearrange("(k p) n -> p k n", p=128)
        slab_ctr = [0]
        sweep_on = [False]

        def load_slab(src3, slots, ncols=512, nk=16, reads=(), nobar=False):
            s = slab_ctr[0] % 3
            slab_ctr[0] += 1
            dst = slots[s][:, 0:nk, 0:ncols]
            if slab_ctr[0] == 1:
                reads = list(reads) + [B_xld]
            P.add("pool", lambda h: h.dma_start(out=dst, in_=src3), reads=list(reads), writes=[B_wsl[s]], chan=ch_wsl[s],
                  nobar=nobar)
            if sweep_on[0]:
                if slab_ctr[0] > 2:
                    emit_conv(2 if slab_ctr[0] <= 10 else 1)
            return s

        fm_ctr = [0]
        psr = [0]

        def next_ps(lo=0, n=4):
            psr[0] = (psr[0] + 1) % n
            return lo + psr[0]

        def proj_fm(c0, ft_base, gate=False, dst=None):
            s = load_slab(w_in_v[:, :, c0:c0 + 512], wsl)
            W = wsl[s]
            for ft in range(4):
                st = fm_ctr[0] % 3
                fm_ctr[0] += 1
                for tc in range(4):
                    pb = next_ps()
                    def mm(h, ft=ft, tc=tc, pb=pb, W=W):
                        for kc in range(KC):
                            ins = h.matmul(ps[pb][:, :], lhsT=W[:, kc, ft * 128:(ft + 1) * 128],
                                           rhs=hT[:, kc, tc * 512:(tc + 1) * 512], start=(kc == 0), stop=(kc == KC - 1))
                        return ins
                    P.add("pe", mm, reads=[B_wsl[s]] + B_hT[4 * tc:4 * tc + 4], writes=[PS[pb]])
                    o_ap = stFM[st][:, tc * 512:(tc + 1) * 512]
                    if gate:
                        P.add("act", lambda h, o_ap=o_ap, pb=pb: h.activation(out=o_ap, in_=ps[pb][:, :], func=AF.Sigmoid),
                              reads=[PS[pb]], writes=[B_stFM[st]])
                    else:
                        evac_copy(o_ap, ps[pb][:, :], [PS[pb]], [B_stFM[st]])
                tgt = (dst if dst is not None else qk_s)[ft_base + ft]
                P.add("sp", lambda h, st=st, tgt=tgt: h.dma_start(out=tgt, in_=stFM[st]),
                      reads=[B_stFM[st]], writes=[B_scr], chan=ch_stFM[st])

        tm_ctr = [0]

        def proj_tm(c0, vslab):
            s = load_slab(w_in_v[:, :, c0:c0 + 512], wsl)
            W = wsl[s]
            for tg in range(4):
                st = tm_ctr[0] % 2
                tm_ctr[0] += 1
                for tl in range(4):
                    tt = 4 * tg + tl
                    pb = next_ps()
                    def mm(h, tt=tt, pb=pb, W=W):
                        for kc in range(KC):
                            ins = h.matmul(ps[pb][:, :], lhsT=hT[:, kc, tt * 128:(tt + 1) * 128], rhs=W[:, kc, :],
                                           start=(kc == 0), stop=(kc == KC - 1))
                        return ins
                    P.add("pe", mm, reads=[B_wsl[s], B_hT[tt]], writes=[PS[pb]])
                    evac_copy(stTM[st][:, tl, :], ps[pb][:, :], [PS[pb]], [B_stTM[st]])
                tgt = v_s[vslab, tg * 512:(tg + 1) * 512, :].rearrange("(a p) c -> p a c", p=128)
                P.add("sp", lambda h, st=st, tgt=tgt: h.dma_start(out=tgt, in_=stTM[st]),
                      reads=[B_stTM[st]], writes=[B_scr], chan=ch_stTM[st])

        B_wbf = Buf("wbf")
        ch_conv = [new_chan(f"ch_conv{i}") for i in range(4)]
        B_cv = [Buf(f"cv{i}") for i in range(4)]
        conv_list = []
        for kc in range(KC):
            conv_list.append((wupb_s[kc * 128:(kc + 1) * 128, :], wup_d[kc * 128:(kc + 1) * 128, :]))
        for kc in range(16):
            conv_list.append((wdnb_s[kc * 512:(kc + 1) * 512, :].rearrange("(p a) n -> p (a n)", a=4),
                              wdn_d[kc * 512:(kc + 1) * 512, :].rearrange("(p a) n -> p (a n)", a=4)))
        conv_ctr = [0]

        def emit_conv(n):
            for _ in range(n):
                if not conv_list:
                    return
                o_ap, i_ap = conv_list.pop(0)
                k = conv_ctr[0] % 4
                conv_ctr[0] += 1
                P.add("pool", lambda h, o_ap=o_ap, i_ap=i_ap: h.dma_start(out=o_ap, in_=i_ap),
                      writes=[B_cv[k]], chan=ch_conv[k])

        B_scr = Buf("scr")

        if phase_limit >= 2:
            sweep_on[0] = True
            for g in range(3):
                proj_fm(C_QKVA + (0 * 3 + g) * 512, FT_AQ + 4 * g)
                proj_fm(C_QKVA + (1 * 3 + g) * 512, FT_AK + 4 * g)
                proj_tm(C_QKVA + (2 * 3 + g) * 512, g)
            for hf in range(2):
                proj_fm(C_QKVB + hf * 512, FT_BQ + 4 * hf)
                proj_fm(C_QKVB + 1024 + hf * 512, FT_BK + 4 * hf)
                proj_tm(C_QKVB + 2048 + hf * 512, 3 + hf)
            for hf in range(2):
                proj_fm(C_QIDX + hf * 512, FT_QI + 4 * hf)
            s = load_slab(w_in_v[:, :, C_KIDX:C_KIDX + 80], wsl, ncols=80)
            W = wsl[s]
            for tt in range(NT):
                pb = next_ps()
                def mm(h, tt=tt, pb=pb, W=W):
                    for kc in range(KC):
                        ins = h.matmul(ps[pb][:, 0:80], lhsT=hT[:, kc, tt * 128:(tt + 1) * 128], rhs=W[:, kc, 0:80],
                                       start=(kc == 0), stop=(kc == KC - 1))
                    return ins
                P.add("pe", mm, reads=[B_wsl[s], B_hT[tt]], writes=[PS[pb]])
                evac_copy(kw[:, tt, :], ps[pb][:, 0:80], [PS[pb]], [B_kw])
            for gi in range(8):
                proj_fm(C_GA + gi * 512, 4 * gi, gate=True, dst=gate_s)
            while conv_list:
                emit_conv(1)
            sweep_on[0] = False
        P.barrier()

        if phase_limit >= 3:
            offs = [128 * (i * (i + 1) // 2) for i in range(NT + 1)]
            scores = carve(8, [offs[NT]], F32)
            kiT = carve(76, [S], BF16)
            kn2 = carve(80, [NT, 128], F32)
            qiT = [carve(88 + 2 * i, [8, 128], BF16) for i in range(2)]
            rbuf = [carve(92 + 2 * i, [512], F32) for i in range(3)] + [carve(112, [512], F32)]
            junk2 = carve(98, [S], BF16)
            nmst = [carve(102 + 4 * i, [S], BF16) for i in range(2)]
            xc = carve(110, [64], F32)
            wsc = carve(111, [NT, 16], F32)
            B_scores = [Buf(f"sc{i}") for i in range(NT)]
            B_kiT, B_kn2, B_xc, B_wsc, B_junk2 = Buf("kiT"), Buf("kn2"), Buf("xc"), Buf("wsc"), Buf("junk2")
            B_qiT = [Buf(f"qiT{i}") for i in range(2)]
            ch_qiT = [new_chan(f"ch_qiT{i}") for i in range(2)]
            B_rbuf = [Buf(f"rbuf{i}") for i in range(4)]
            rbufP = [carve(114 + 2 * i, [512], F32) for i in range(4)]
            tmpP = carve(122, [512], F32)
            accP = [carve(124 + 2 * i, [512], F32) for i in range(2)]
            B_rbufP = [Buf(f"rbufP{i}") for i in range(4)]
            B_tmpP = Buf("tmpP")
            B_accP = [Buf("accP0"), Buf("accP1")]
            B_nmst = [Buf(f"nmst{i}") for i in range(2)]
            ch_nmst = [new_chan(f"ch_nmst{i}") for i in range(2)]
            B_nmscr = Buf("nmscr")

            for i in range(NT):
                kv = kw[:, i, 0:64]
                P.add("act", lambda h, i=i, kv=kv: h.activation(out=xc, in_=kv, func=AF.Identity,
                                                                accum_out=SM[:, c_s1 + i:c_s1 + i + 1]),
                      reads=[B_kw], writes=[B_xc, B_sm])
                P.add("dve", lambda h, i=i: h.tensor_scalar(out=SM[:, c_negmu + i:c_negmu + i + 1],
                                                            in0=SM[:, c_s1 + i:c_s1 + i + 1], scalar1=-1.0 / 64,
                                                            scalar2=None, op0=ALU.mult), reads=[B_sm], writes=[B_sm])
                P.add("act", lambda h, i=i, kv=kv: h.activation(out=kn2[:, i, 0:64], in_=kv, func=AF.Identity,
                                                                bias=SM[:, c_negmu + i:c_negmu + i + 1], scale=1.0),
                      reads=[B_kw, B_sm], writes=[B_kn2])
                P.add("act", lambda h, i=i: h.activation(out=xc, in_=kn2[:, i, 0:64], func=AF.Square,
                                                         accum_out=SM[:, c_s2 + i:c_s2 + i + 1]),
                      reads=[B_kn2], writes=[B_xc, B_sm])
            rstd_ops(c_s2, NT, c_sd, c_rs, 1.0 / 64)
            for i in range(NT):
                P.add("dve", lambda h, i=i: h.tensor_scalar(out=kn2[:, i, 64:128], in0=kn2[:, i, 0:64],
                                                            scalar1=SM[:, c_rs + i:c_rs + i + 1], scalar2=None,
                                                            op0=ALU.mult), reads=[B_kn2, B_sm], writes=[B_kn2])
                P.add("dve", lambda h, i=i: h.tensor_copy(out=kn2[:, i, 0:64], in_=kn2[:, i, 64:128]),
                      reads=[B_kn2], writes=[B_kn2])
                pb = next_ps()
                P.add("pe", lambda h, i=i, pb=pb: h.transpose(out=ps[pb][:, 0:128], in_=kn2[:, i, :], identity=identF[:]),
                      reads=[B_kn2, B_const], writes=[PS[pb]])
                P.add("act", lambda h, i=i, pb=pb: h.activation(out=kiT[:, i * 128:(i + 1) * 128], in_=ps[pb][:, 0:128],
                                                                func=AF.Identity, scale=SM[:, c_ikg:c_ikg + 1],
                                                                bias=SM[:, c_ikb:c_ikb + 1]),
                      reads=[PS[pb], B_const], writes=[B_kiT])
            P.add("dve", lambda h: h.tensor_scalar(out=wsc, in0=kw[:, :, 64:80], scalar1=2.0 ** -5, scalar2=None,
                                                   op0=ALU.mult), reads=[B_kw], writes=[B_wsc])

            P.add("pool", lambda h: h.memset(nmst[0][:, 0:256], 0.0), writes=[B_nmst[0]])
            for i in range(2):
                P.add("sp", lambda h, i=i: h.dma_start(out=nm_s[i, :, 0:128 * (i + 1)], in_=nmst[0][:, 0:128 * (i + 1)]),
                      reads=[B_nmst[0]], writes=[B_nmscr], chan=ch_nmst[0])

            qi_src = qk_s[FT_QI:FT_QI + 8].rearrange("f p c -> p f c")
            for i in range(2, NT):
                sl = i % 2
                L = 128 * (i + 1)
                P.add("sp", lambda h, i=i, sl=sl: h.dma_start(out=qiT[sl], in_=qi_src[:, :, i * 128:(i + 1) * 128]),
                      writes=[B_qiT[sl]], chan=ch_qiT[sl])
                nch = (L + 511) // 512
                for c in range(nch):
                    wc = min(512, L - 512 * c)
                    pacc0 = 4 + 2 * (c % 2)
                    accp = accP[c % 2]
                    B_accp = B_accP[c % 2]
                    n_dve = 0
                    for hh in (0, 1, 2, 12, 3, 4, 5, 13, 6, 7, 8, 14, 9, 10, 11, 15):
                        po = (hh % 2) * 64
                        pb = next_ps(0, 3)
                        on_pool = hh >= 12
                        if on_pool:
                            rtile, B_rt = rbufP[hh - 12], B_rbufP[hh - 12]
                        else:
                            rtile, B_rt = rbuf[n_dve % 3], B_rbuf[n_dve % 3]
                        P.add("pe", lambda h, hh=hh, po=po, pb=pb, sl=sl, c=c, wc=wc: h.matmul(
                            ps[pb][:, 0:wc], lhsT=qiT[sl][po:po + 64, hh // 2, :],
                            rhs=kiT[po:po + 64, c * 512:c * 512 + wc], start=True, stop=True),
                            reads=[B_qiT[sl], B_kiT], writes=[PS[pb]])
                        P.add("act", lambda h, pb=pb, rtile=rtile, wc=wc: h.activation(out=rtile[:, 0:wc], in_=ps[pb][:, 0:wc],
                                                                                       func=AF.Relu),
                              reads=[PS[pb]], writes=[B_rt])
                        wcol = wsc[:, i, hh:hh + 1]
                        if on_pool:
                            if hh == 12:
                                P.add("pool", lambda h, rtile=rtile, wc=wc, wcol=wcol, accp=accp: h.tensor_scalar(
                                    out=accp[:, 0:wc], in0=rtile[:, 0:wc], scalar1=wcol, scalar2=0.0, op0=ALU.mult,
                                    op1=ALU.add), reads=[B_rt, B_wsc], writes=[B_accp])
                            else:
                                P.add("pool", lambda h, rtile=rtile, wc=wc, wcol=wcol: h.tensor_scalar(
                                    out=tmpP[:, 0:wc], in0=rtile[:, 0:wc], scalar1=wcol, scalar2=0.0, op0=ALU.mult,
                                    op1=ALU.add), reads=[B_rt, B_wsc], writes=[B_tmpP])
                                P.add("pool", lambda h, wc=wc, accp=accp: h.tensor_tensor(
                                    out=accp[:, 0:wc], in0=accp[:, 0:wc], in1=tmpP[:, 0:wc], op=ALU.add),
                                    reads=[B_tmpP, B_accp], writes=[B_accp])
                            continue
                        pacc = pacc0 + (n_dve % 2)
                        if n_dve < 2:
                            P.add("dve", lambda h, rtile=rtile, wc=wc, pacc=pacc, wcol=wcol: h.tensor_scalar(
                                out=ps[pacc][:, 0:wc], in0=rtile[:, 0:wc], scalar1=wcol, scalar2=None, op0=ALU.mult),
                                reads=[B_rt, B_wsc], writes=[PS[pacc]])
                        else:
                            P.add("dve", lambda h, rtile=rtile, wc=wc, pacc=pacc, wcol=wcol: h.scalar_tensor_tensor(
                                out=ps[pacc][:, 0:wc], in0=rtile[:, 0:wc], scalar=wcol, in1=ps[pacc][:, 0:wc],
                                op0=ALU.mult, op1=ALU.add), reads=[B_rt, B_wsc, PS[pacc]], writes=[PS[pacc]])
                        n_dve += 1
                    P.add("act", lambda h, wc=wc, pacc0=pacc0: h.activation(
                        out=rbuf[3][:, 0:wc], in_=ps[pacc0 + 1][:, 0:wc], func=AF.Copy),
                        reads=[PS[pacc0 + 1]], writes=[B_rbuf[3]])
                    sc_ap = scores[:, offs[i] + 512 * c:offs[i] + 512 * c + wc]
                    P.add("dve", lambda h, sc_ap=sc_ap, wc=wc, pacc0=pacc0: h.tensor_tensor(
                        out=sc_ap, in0=ps[pacc0][:, 0:wc], in1=rbuf[3][:, 0:wc], op=ALU.add),
                        reads=[PS[pacc0], B_rbuf[3]], writes=[B_scores[i]])
                    P.add("pool", lambda h, sc_ap=sc_ap, wc=wc, accp=accp: h.tensor_tensor(
                        out=sc_ap, in0=sc_ap, in1=accp[:, 0:wc], op=ALU.add),
                        reads=[B_scores[i], B_accp], writes=[B_scores[i]])
                P.add("pool", lambda h, i=i: h.affine_select(
                    out=scores[:, offs[i] + 128 * i:offs[i] + 128 * (i + 1)],
                    in_=scores[:, offs[i] + 128 * i:offs[i] + 128 * (i + 1)], pattern=[[-1, 128]],
                    compare_op=ALU.is_ge, fill=-1e30, base=0, channel_multiplier=1),
                    reads=[B_scores[i]], writes=[B_scores[i]])
                P.add("dve", lambda h, i=i, L=L: h.tensor_reduce(out=SM[:, c_hi + i:c_hi + i + 1],
                                                                 in_=scores[:, offs[i]:offs[i] + L], axis=AX.X, op=ALU.max),
                      reads=[B_scores[i]], writes=[B_sm])
                P.add("dve", lambda h, i=i: h.tensor_reduce(out=SM[:, c_lo + i:c_lo + i + 1],
                                                            in_=scores[:, offs[i]:offs[i] + 128 * i], axis=AX.X, op=ALU.min),
                      reads=[B_scores[i]], writes=[B_sm])
        P.barrier()

        OA = [carve(140 + 4 * i, [S], BF16) for i in range(4)]
        OB = [carve(156 + 4 * i, [S], BF16) for i in range(8)]
        B_OA = [Buf(f"OA{i}") for i in range(4)]
        B_OB = [Buf(f"OB{i}") for i in range(8)]

        def bisect_gen():
            junk2 = carve(76, [S], BF16)
            nmst = [carve(80 + 4 * i, [S], BF16) for i in range(2)]
            B_junk2 = Buf("junk2b")
            B_nmst = [Buf(f"nmstb{i}") for i in range(2)]
            T0, T1 = 2, NT
            P.add("dve", lambda h: h.tensor_tensor(out=SM[:, c_w0 + T0:c_w0 + T1], in0=SM[:, c_hi + T0:c_hi + T1],
                                                   in1=SM[:, c_lo + T0:c_lo + T1], op=ALU.subtract),
                  reads=[B_sm], writes=[B_sm])
            P.add("dve", lambda h: h.tensor_scalar(out=SM[:, c_w0 + T0:c_w0 + T1], in0=SM[:, c_w0 + T0:c_w0 + T1],
                                                   scalar1=1.001, scalar2=1e-6, op0=ALU.mult, op1=ALU.add),
                  reads=[B_sm], writes=[B_sm])
            for i in range(T0, T1):
                P.add("dve", lambda h, i=i: h.tensor_scalar(out=SM[:, c_steps + i * NIT:c_steps + (i + 1) * NIT],
                                                            in0=SM[:, c_pw2:c_pw2 + NIT], scalar1=SM[:, c_w0 + i:c_w0 + i + 1],
                                                            scalar2=None, op0=ALU.mult), reads=[B_sm, B_const], writes=[B_sm])
            yield 2.0
            steps_v = SM[:, c_steps:c_steps + NT * NIT].rearrange("p (a b) -> p a b", b=NIT)
            for k in range(NIT):
                stepk = steps_v[:, T0:T1, k]
                P.add("dve", lambda h, stepk=stepk: h.tensor_tensor(out=SM[:, c_mid + T0:c_mid + T1],
                                                                    in0=SM[:, c_lo + T0:c_lo + T1], in1=stepk, op=ALU.add),
                      reads=[B_sm], writes=[B_sm])
                for i in range(T0, T1):
                    L = 128 * (i + 1)
                    P.add("dve", lambda h, i=i, L=L: h.tensor_scalar(
                        out=junk2[:, 0:L], in0=scores[:, offs[i]:offs[i] + L], scalar1=SM[:, c_mid + i:c_mid + i + 1],
                        scalar2=None, op0=ALU.is_ge, op1=ALU.add, accum_out=SM[:, c_cnt + i:c_cnt + i + 1]),
                        reads=[B_scores[i], B_sm], writes=[B_junk2, B_sm])
                    yield L / 960.0 + 0.15
                P.add("dve", lambda h, stepk=stepk: h.scalar_tensor_tensor(
                    out=SM[:, c_dd + T0:c_dd + T1], in0=SM[:, c_cnt + T0:c_cnt + T1], scalar=TOPK - 0.5, in1=stepk,
                    op0=ALU.is_gt, op1=ALU.mult), reads=[B_sm], writes=[B_sm])
                P.add("dve", lambda h: h.tensor_tensor(out=SM[:, c_lo + T0:c_lo + T1], in0=SM[:, c_lo + T0:c_lo + T1],
                                                       in1=SM[:, c_dd + T0:c_dd + T1], op=ALU.add),
                      reads=[B_sm], writes=[B_sm])
                yield 0.5
            for i in range(T0, T1):
                L = 128 * (i + 1)
                sl = i % 2
                P.add("dve", lambda h, i=i, L=L, sl=sl: h.tensor_scalar(
                    out=nmst[sl][:, 0:L], in0=scores[:, offs[i]:offs[i] + L], scalar1=SM[:, c_lo + i:c_lo + i + 1],
                    scalar2=NMV, op0=ALU.is_lt, op1=ALU.mult), reads=[B_scores[i], B_sm], writes=[B_nmst[sl]])
                P.add("sp", lambda h, i=i, L=L, sl=sl: h.dma_start(out=nm_s[i, :, 0:L], in_=nmst[sl][:, 0:L]),
                      reads=[B_nmst[sl]], writes=[B_nmscr], chan=ch_nmst[sl])
                yield L / 960.0 + 0.15

        def merge_threads(gens):
            t = [0.0] * len(gens)
            live = list(range(len(gens)))
            while live:
                k = min(live, key=lambda a: t[a])
                try:
                    t[k] += next(gens[k])
                except StopIteration:
                    live.remove(k)

        ch_odump = new_chan("ch_odump", exact=False)
        e_ctr = [0]
        nmc_ctr = [0]

        def attention_job(subs, is_b, o_ap, B_o, dump_idx, bs, Ebuf, B_E, rden, B_rden, NMc=None, B_NMc=None, ch_NMc=None,
                          pc=None):
            JQ, JK, JV, JS, B_J, ch_J = bs
            if pc is not None:
                for gi, (qt, kt, vs, vc, row, win) in enumerate(subs):
                    ssrc = bass.AP(tensor=r_t, offset=row * MROW, ap=[[1, 128], [1, WSTRIP]])
                    P.add("sp", lambda h, gi=gi, ssrc=ssrc: h.dma_start(out=JS[gi], in_=ssrc), writes=[pc["S"][gi]],
                          chan=pc["ch"][0])
                for p_ in range(4):
                    cs = slice(p_ * 512, (p_ + 1) * 512)
                    for gi, (qt, kt, vs, vc, row, win) in enumerate(subs):
                        P.add("sp", lambda h, gi=gi, qt=qt, cs=cs: h.dma_start(out=JQ[gi][:, cs], in_=qk_s[qt][:, cs]),
                              writes=[pc["Q"][gi][p_]], chan=pc["ch"][p_])
                        P.add("sp", lambda h, gi=gi, kt=kt, cs=cs: h.dma_start(out=JK[gi][:, cs], in_=qk_s[kt][:, cs]),
                              writes=[pc["K"][gi][p_]], chan=pc["ch"][p_])
                        vsrc = v_s[vs, cs, vc:vc + 128].rearrange("(a p) c -> p a c", p=128)
                        P.add("sp", lambda h, gi=gi, vsrc=vsrc, p_=p_: h.dma_start(out=JV[gi][:, 4 * p_:4 * p_ + 4, :], in_=vsrc),
                              writes=[pc["V"][gi][p_]], chan=pc["ch"][p_])
            for gi, (qt, kt, vs, vc, row, win) in (enumerate(subs) if pc is None else ()):
                P.add("sp", lambda h, gi=gi, qt=qt: h.dma_start(out=JQ[gi], in_=qk_s[qt]), writes=[B_J[gi]], chan=ch_J[gi])
                P.add("sp", lambda h, gi=gi, kt=kt: h.dma_start(out=JK[gi], in_=qk_s[kt]), writes=[B_J[gi]], chan=ch_J[gi])
                vsrc = v_s[vs, :, vc:vc + 128].rearrange("(a p) c -> p a c", p=128)
                P.add("sp", lambda h, gi=gi, vsrc=vsrc: h.dma_start(out=JV[gi], in_=vsrc), writes=[B_J[gi]], chan=ch_J[gi])
                ssrc = bass.AP(tensor=r_t, offset=row * MROW, ap=[[1, 128], [1, WSTRIP]])
                P.add("sp", lambda h, gi=gi, ssrc=ssrc: h.dma_start(out=JS[gi], in_=ssrc), writes=[B_J[gi]], chan=ch_J[gi])
            pending = [None]

            def chunk_gen(c):
                pnum = 3 + (c % 2)
                pden = 5 + (c % 2)
                ns = 0
                if is_b:
                    ns = nmc_ctr[0] % 2
                    nmc_ctr[0] += 1
                    nsrc = nm_s[4 * c:4 * c + 4].rearrange("a p s -> p a s")
                    P.add("sp", lambda h, ns=ns, nsrc=nsrc: h.dma_start(out=NMc[ns], in_=nsrc), reads=[B_nmscr],
                          writes=[B_NMc[ns]], chan=ch_NMc[ns])
                units = []
                for gi, (qt, kt, vs, vc, row, win) in enumerate(subs):
                    jlo = max(0, -((-(512 * c - 127 - win)) // 128))
                    for j in range(jlo, 4 * c + 4):
                        units.append((gi, j))

                def qk(u):
                    gi, j = units[u]
                    pb = u % 3
                    xoff = 512 * c - 128 * j + 384
                    tl = [il for il in range(4) if 4 * c + il >= max(j, 2)] if is_b else []

                    def f(h):
                        h.matmul(ps[pb][:, :], lhsT=JK[gi][:, j * 128:(j + 1) * 128],
                                 rhs=JQ[gi][:, c * 512:(c + 1) * 512], start=True, stop=False)
                        ins = h.matmul(ps[pb][:, :], lhsT=antiJ[:], rhs=JS[gi][:, xoff:xoff + 512], start=False,
                                       stop=(len(tl) == 0))
                        for n_, il in enumerate(tl):
                            ins = h.matmul(ps[pb][:, il * 128:(il + 1) * 128],
                                           lhsT=NMc[ns][:, il, j * 128:(j + 1) * 128], rhs=identB[:],
                                           start=False, stop=(n_ == len(tl) - 1))
                        return ins
                    if pc is not None:
                        rd = [pc["K"][gi][j // 4], pc["Q"][gi][c], pc["S"][gi], B_const]
                    else:
                        rd = [B_J[gi], B_const] + ([B_NMc[ns]] if is_b else [])
                    P.add("pe", f, reads=rd, writes=[PS[pb]])

                def ex(u):
                    pb = u % 3
                    eb = e_ctr[0] % 3
                    e_ctr[0] += 1
                    P.add("act", lambda h: h.activation(out=Ebuf[eb], in_=ps[pb][:, :], func=AF.Exp, scale=SCALE),
                          reads=[PS[pb]], writes=[B_E[eb]])
                    return eb

                def pv(u, eb):
                    gi, j = units[u]
                    first = (u == 0)
                    last = (u == len(units) - 1)

                    def f(h):
                        h.matmul(ps[pnum][:, :], lhsT=JV[gi][:, j, :], rhs=Ebuf[eb], start=first, stop=last)
                        return h.matmul(ps[pden][:, :], lhsT=onesB[:], rhs=Ebuf[eb], start=first, stop=last)
                    P.add("pe", f, reads=[(pc["V"][gi][j // 4] if pc is not None else B_J[gi]), B_E[eb], B_const],
                          writes=[PS[pnum], PS[pden]])

                def norm():
                    P.add("dve", lambda h: h.reciprocal(out=rden, in_=ps[pden][:, :]), reads=[PS[pden]], writes=[B_rden])
                    P.add("dve", lambda h: h.tensor_tensor(out=o_ap[:, c * 512:(c + 1) * 512], in0=ps[pnum][:, :], in1=rden,
                                                           op=ALU.mult), reads=[PS[pnum], B_rden], writes=[B_o])

                qk(0)
                for u in range(len(units)):
                    if u + 1 < len(units):
                        qk(u + 1)
                    eb = ex(u)
                    pv(u, eb)
                    if u == 2 and pending[0] is not None:
                        pending[0]()
                        pending[0] = None
                    yield 1.4 if is_b else 1.2
                pending[0] = norm

            for c in range(4):
                yield from chunk_gen(c)
            pending[0]()
            if debug:
                P.add("sp", lambda h: h.dma_start(out=o_s[dump_idx], in_=o_ap), reads=[B_o], writes=[Buf("dump")],
                      chan=ch_odump)

        if phase_limit >= 4:
            bsA = ([carve(88 + 4 * i, [S], BF16) for i in range(3)], [carve(100 + 4 * i, [S], BF16) for i in range(3)],
                   [carve(112 + 4 * i, [NT, 128], BF16) for i in range(3)], [carve(124 + 5 * i, [WSTRIP], BF16) for i in range(3)],
                   [Buf(f"JA{i}") for i in range(3)], [new_chan(f"ch_JA{i}", exact=False) for i in range(3)])
            pcA = {"Q": [[Buf(f"JAQ{g}_{p}") for p in range(4)] for g in range(3)],
                   "K": [[Buf(f"JAK{g}_{p}") for p in range(4)] for g in range(3)],
                   "V": [[Buf(f"JAV{g}_{p}") for p in range(4)] for g in range(3)],
                   "S": [Buf(f"JAS{g}") for g in range(3)],
                   "ch": bsA[5] + [ch_odump]}
            EbufA = [carve(0 + i, [512], BF16) for i in range(3)]
            rdenA = carve(4, [512], F32)
            B_EA = [Buf(f"EA{i}") for i in range(3)]
            B_rdenA = Buf("rdenA")

            def a_thread():
                for hh in range(4):
                    subs = [(FT_AQ + 4 * g + hh, FT_AK + 4 * g + hh, g, hh * 128, 4 * g + hh, WINS[g]) for g in range(3)]
                    yield from attention_job(subs, False, OA[hh], B_OA[hh], hh, bsA, EbufA, B_EA, rdenA, B_rdenA, pc=pcA)

            merge_threads([a_thread(), bisect_gen()])
            P.barrier()
            WPA = carve(76, [4, D], BF16)
            WPB = carve(92, [8, D], BF16)
            B_WP = Buf("WP")
            ch_WP = new_chan("ch_WP", exact=False)
            P.add("pool", lambda h: h.dma_start(out=WPA, in_=wpa_d.rearrange("(a p) n -> p a n", p=128)), writes=[B_WP],
                  chan=ch_WP, nobar="out")
            for hf in range(2):
                P.add("pool", lambda h, hf=hf: h.dma_start(out=WPB[:, 4 * hf:4 * hf + 4, :],
                                                           in_=wpb_d[512 * hf:512 * hf + 512, :].rearrange("(a p) n -> p a n", p=128)),
                      writes=[B_WP], chan=ch_WP, nobar="out")
            bsB = []
            for k in range(2):
                o = 20 * k
                bsB.append(([carve(o, [S], BF16)], [carve(o + 4, [S], BF16)], [carve(o + 8, [NT, 128], BF16)],
                            [carve(o + 12, [WSTRIP], BF16)], [Buf(f"JB{k}")], [new_chan(f"ch_JB{k}")]))
            NMc = [carve(37 + 16 * i, [4, S], BF16) for i in range(2)]
            B_NMc = [Buf(f"NMc{i}") for i in range(2)]
            ch_NMc = [new_chan(f"ch_NMc{i}") for i in range(2)]
            EbufB = [carve(69 + i, [512], BF16) for i in range(3)]
            rdenB = carve(72, [512], F32)
            B_EB = [Buf(f"EB{i}") for i in range(3)]
            B_rdenB = Buf("rdenB")

            def b_thread():
                for hb_ in range(8):
                    subs = [(FT_BQ + hb_, FT_BK + hb_, 3 + hb_ // 4, (hb_ % 4) * 128, 12 + hb_, 1 << 30)]
                    yield from attention_job(subs, True, OB[hb_], B_OB[hb_], 4 + hb_, bsB[hb_ % 2], EbufB, B_EB, rdenB,
                                             B_rdenB, NMc, B_NMc, ch_NMc)

            merge_threads([b_thread()])
        P.barrier()

        mergedT = carve(0, [KC, S], BF16)
        B_mg = [Buf(f"mg{i}") for i in range(KC)]
        if phase_limit >= 5:
            sga2 = [carve(124, [S], BF16), carve(64, [S], BF16)]
            sgb2 = [carve(128, [S], BF16), carve(68, [S], BF16)]
            tmpa = [carve(132 + 2 * i, [512], F32) for i in range(2)]
            tmpb = [carve(136 + 2 * i, [512], F32) for i in range(2)]
            B_sga2, B_sgb2 = [Buf("sga0"), Buf("sga1")], [Buf("sgb0"), Buf("sgb1")]
            ch_sga2 = [new_chan("ch_sga0"), new_chan("ch_sga1")]
            ch_sgb2 = [new_chan("ch_sgb0"), new_chan("ch_sgb1")]
            B_tmpa = [Buf(f"tmpa{i}") for i in range(2)]
            B_tmpb = [Buf(f"tmpb{i}") for i in range(2)]
            u_ctr = 0
            for dt in range(KC):
                sga, sgb = sga2[dt % 2], sgb2[dt % 2]
                B_sga, B_sgb = B_sga2[dt % 2], B_sgb2[dt % 2]
                P.add("sp", lambda h, dt=dt, sga=sga: h.dma_start(out=sga, in_=gate_s[dt]), writes=[B_sga],
                      chan=ch_sga2[dt % 2])
                P.add("sp", lambda h, dt=dt, sgb=sgb: h.dma_start(out=sgb, in_=gate_s[16 + dt]), writes=[B_sgb],
                      chan=ch_sgb2[dt % 2])
                for tc in range(4):
                    pa = 2 * (u_ctr % 2)
                    pbk = pa + 1
                    tsl = u_ctr % 2
                    u_ctr += 1
                    def mma(h, dt=dt, tc=tc, pa=pa):
                        for a in range(4):
                            ins = h.matmul(ps[pa][:, :], lhsT=WPA[:, a, dt * 128:(dt + 1) * 128],
                                           rhs=OA[a][:, tc * 512:(tc + 1) * 512], start=(a == 0), stop=(a == 3))
                        return ins
                    P.add("pe", mma, reads=[B_WP] + B_OA, writes=[PS[pa]])
                    def mmb(h, dt=dt, tc=tc, pbk=pbk):
                        for a in range(8):
                            ins = h.matmul(ps[pbk][:, :], lhsT=WPB[:, a, dt * 128:(dt + 1) * 128],
                                           rhs=OB[a][:, tc * 512:(tc + 1) * 512], start=(a == 0), stop=(a == 7))
                        return ins
                    P.add("pe", mmb, reads=[B_WP] + B_OB, writes=[PS[pbk]])
                    P.add("dve", lambda h, pa=pa, tc=tc, tsl=tsl, sga=sga: h.tensor_tensor(
                        out=tmpa[tsl], in0=ps[pa][:, :], in1=sga[:, tc * 512:(tc + 1) * 512], op=ALU.mult),
                        reads=[PS[pa], B_sga], writes=[B_tmpa[tsl]])
                    P.add("dve", lambda h, pbk=pbk, tc=tc, tsl=tsl, sgb=sgb: h.tensor_tensor(
                        out=tmpb[tsl], in0=ps[pbk][:, :], in1=sgb[:, tc * 512:(tc + 1) * 512], op=ALU.mult),
                        reads=[PS[pbk], B_sgb], writes=[B_tmpb[tsl]])
                    P.add("pool", lambda h, dt=dt, tc=tc, tsl=tsl: h.tensor_tensor(
                        out=mergedT[:, dt, tc * 512:(tc + 1) * 512], in0=tmpa[tsl], in1=tmpb[tsl], op=ALU.add),
                        reads=[B_tmpa[tsl], B_tmpb[tsl]], writes=[B_mg[dt]])
        P.barrier()

        B_x1scr = Buf("x1scr")
        if phase_limit >= 6:
            wsl5 = [carve(72 + 16 * i, [16, 512], BF16) for i in range(3)]
            xin = [carve(120 + 2 * i, [512], F32) for i in range(4)]
            x1p = [carve(128 + 2 * i, [512], F32) for i in range(4)]
            junk5 = carve(136, [512], BF16)
            B_xin = [Buf(f"xin{i}") for i in range(4)]
            ch_xin = [new_chan(f"ch_xin{i}") for i in range(4)]
            B_x1p = [Buf(f"x1p{i}") for i in range(4)]
            ch_x1p = [new_chan(f"ch_x1p{i}") for i in range(4)]
            B_junk5 = Buf("junk5")
            wout_v = wout_d.rearrange("(k p) n -> p k n", p=128)
            def xin_load(uu):
                dc_, i_ = divmod(uu, NT)
                sl_ = uu % 4
                P.add("sp", lambda h: h.dma_start(out=xin[sl_], in_=x_d[i_ * 128:(i_ + 1) * 128, dc_ * 512:(dc_ + 1) * 512]),
                      writes=[B_xin[sl_]], chan=ch_xin[sl_])

            xin_load(0)
            xin_load(1)
            u = 0
            for dc in range(4):
                s = load_slab(wout_v[:, :, dc * 512:(dc + 1) * 512], wsl5)
                W = wsl5[s]
                for i in range(NT):
                    sl = u % 4
                    if u + 2 < 4 * NT:
                        xin_load(u + 2)
                    u += 1
                    pb = next_ps()
                    def mm(h, i=i, W=W, pb=pb):
                        for kc in range(KC):
                            ins = h.matmul(ps[pb][:, :], lhsT=mergedT[:, kc, i * 128:(i + 1) * 128], rhs=W[:, kc, :],
                                           start=(kc == 0), stop=(kc == KC - 1))
                        return ins
                    P.add("pe", mm, reads=[B_wsl[s]] + B_mg, writes=[PS[pb]])
                    P.add("dve", lambda h, pb=pb, sl=sl: h.tensor_tensor(out=x1p[sl], in0=ps[pb][:, :], in1=xin[sl],
                                                                         op=ALU.add),
                          reads=[PS[pb], B_xin[sl]], writes=[B_x1p[sl]])
                    P.add("act", lambda h, i=i, dc=dc, sl=sl: h.activation(
                        out=junk5, in_=x1p[sl], func=AF.Square, accum_out=SM[:, c_ssq1 + 4 * i + dc:c_ssq1 + 4 * i + dc + 1]),
                        reads=[B_x1p[sl]], writes=[B_junk5, B_sm])
                    P.add("sp", lambda h, i=i, dc=dc, sl=sl: h.dma_start(
                        out=x1_s[i * 128:(i + 1) * 128, dc * 512:(dc + 1) * 512], in_=x1p[sl]), reads=[B_x1p[sl]],
                        writes=[B_x1scr], chan=ch_x1p[sl])
        P.barrier()

        ch_out = new_chan("ch_out", exact=False)
        P.out_chans.append(ch_out)
        if phase_limit >= 7:
            hmT2 = [carve(0, [KC, 512], BF16), carve(56, [KC, 512], BF16)]
            actT = carve(120, [64, 512], BF16)
            wsl6 = [carve(72 + 16 * i, [16, 512], BF16) for i in range(3)]
            junk6_ap = carve(188, [512], BF16)
            x1t = [carve(16 + 8 * i, [D], F32) for i in range(4)]
            gfin = carve(48, [D], F32)
            hb6 = carve(190, [D], F32)
            sq = [carve(184 + 2 * i, [512], F32) for i in range(2)]
            B_hmT2, B_gf, B_hb6 = [Buf("hmT0"), Buf("hmT1")], Buf("gfin"), Buf("hb6")
            ch_hb6 = new_chan("ch_hb6")
            B_actT = [Buf(f"actT{i}") for i in range(64)]
            B_x1t = [Buf(f"x1t{i}") for i in range(4)]
            ch_x1t = [new_chan(f"ch_x1t{i}") for i in range(4)]
            B_sq = [Buf(f"sq{i}") for i in range(2)]
            B_junk6 = Buf("junk6")
            B_gT = Buf("gT")
            wup_v = wupb_s.rearrange("(k p) n -> p k n", p=128)
            wdn_v = wdnb_s.rearrange("(k p) n -> p k n", p=128)
            P.add("sp", lambda h: h.dma_start(out=gfin, in_=gfin_d.partition_broadcast(128)), writes=[B_gf], chan=ch_misc)
            g16 = carve(199, [128], F32)
            B_g16 = Buf("g16")
            P.add("sp", lambda h: h.dma_start(out=g16[0:16, :], in_=gmlp_d.rearrange("(k p) -> k p", p=128)),
                  writes=[B_g16], chan=ch_misc)
            P.add("pe", lambda h: h.transpose(out=ps[7][:, 0:16], in_=g16[0:16, :], identity=identF[0:16, 0:16]),
                  reads=[B_g16, B_const], writes=[PS[7]])
            P.add("dve", lambda h: h.tensor_copy(out=SM[:, c_gT:c_gT + KC], in_=ps[7][:, 0:16]), reads=[PS[7]],
                  writes=[B_gT])
            ssq1_v = SM[:, c_ssq1:c_ssq1 + 64].rearrange("p (a b) -> p a b", b=4)
            P.add("dve", lambda h: h.tensor_reduce(out=SM[:, c_t1:c_t1 + 16], in_=ssq1_v, axis=AX.X, op=ALU.add),
                  reads=[B_sm], writes=[B_sm])
            rstd_ops(c_t1, 16, c_rt, c_rstd, 1.0 / D)
            prep_ps = [0]

            def prep_a(tc_, il):
                i = 4 * tc_ + il
                P.add("sp", lambda h: h.dma_start(out=hb6, in_=x1_s[i * 128:(i + 1) * 128, :]), reads=[B_x1scr],
                      writes=[B_hb6], chan=ch_hb6)
                P.add("act", lambda h: h.activation(out=hb6, in_=hb6, func=AF.Identity,
                                                    scale=SM[:, c_rstd + i:c_rstd + i + 1]),
                      reads=[B_hb6, B_sm], writes=[B_hb6])

            def prep_b(tc_, il):
                hm = hmT2[tc_ % 2]
                for q in range(4):
                    pb = 4 + prep_ps[0] % 4
                    prep_ps[0] += 1
                    def tr(h, q=q, pb=pb):
                        for j in range(4):
                            kc = 4 * q + j
                            ins = h.transpose(out=ps[pb][:, j * 128:(j + 1) * 128], in_=hb6[:, kc * 128:(kc + 1) * 128],
                                              identity=identF[:])
                        return ins
                    P.add("pe", tr, reads=[B_hb6, B_const], writes=[PS[pb]])
                    for j in range(4):
                        kc = 4 * q + j
                        evac_copy(hm[:, kc, il * 128:(il + 1) * 128], ps[pb][:, j * 128:(j + 1) * 128], [PS[pb], B_gT],
                                  [B_hmT2[tc_ % 2]], scale=SM[:, c_gT + kc:c_gT + kc + 1])

            def x1t_load(tc_, il):
                i = 4 * tc_ + il
                P.add("sp", lambda h: h.dma_start(out=x1t[il], in_=x1_s[i * 128:(i + 1) * 128, :]),
                      reads=[B_x1scr], writes=[B_x1t[il]], chan=ch_x1t[il])

            for il in range(4):
                prep_a(0, il)
                prep_b(0, il)
            uq = 0
            for tcc in range(4):
                hmT = hmT2[tcc % 2]
                B_hmT = B_hmT2[tcc % 2]
                for il in range(4):
                    x1t_load(tcc, il)
                for su in range(16):
                    s = load_slab(wup_v[:, :, su * 512:(su + 1) * 512], wsl6, reads=B_cv, nobar=("in" if (tcc == 0 and su < 3) else False))
                    W = wsl6[s]
                    for ft in range(4):
                        f_idx = 4 * su + ft
                        if tcc + 1 < 4:
                            if f_idx % 16 == 0:
                                prep_a(tcc + 1, f_idx // 16)
                            elif f_idx % 16 == 8:
                                prep_b(tcc + 1, f_idx // 16)
                        pb = next_ps()
                        qs = uq % 2
                        uq += 1
                        def mm(h, ft=ft, W=W, pb=pb, hmT=hmT):
                            for kc in range(KC):
                                ins = h.matmul(ps[pb][:, :], lhsT=W[:, kc, ft * 128:(ft + 1) * 128], rhs=hmT[:, kc, :],
                                               start=(kc == 0), stop=(kc == KC - 1))
                            return ins
                        P.add("pe", mm, reads=[B_wsl[s], B_hmT], writes=[PS[pb]])
                        P.add("act", lambda h, pb=pb, qs=qs: h.activation(out=sq[qs], in_=ps[pb][:, :], func=AF.Square),
                              reads=[PS[pb]], writes=[B_sq[qs]])
                        P.add("dve", lambda h, pb=pb, qs=qs, f_idx=f_idx: h.scalar_tensor_tensor(
                            out=actT[:, f_idx, :], in0=ps[pb][:, :], scalar=0.0, in1=sq[qs], op0=ALU.is_gt, op1=ALU.mult),
                            reads=[PS[pb], B_sq[qs]], writes=[B_actT[f_idx]])
                for dc in range(4):
                    pbase = 4 * (dc % 2)
                    for sd in range(4):
                        s = load_slab(wdn_v[:, 16 * sd:16 * sd + 16, dc * 512:(dc + 1) * 512], wsl6, reads=B_cv)
                        W = wsl6[s]
                        def mm(h, sd=sd, W=W, pbase=pbase):
                            for fl in range(16):
                                fc = 16 * sd + fl
                                for il in range(4):
                                    ins = h.matmul(ps[pbase + il][:, :], lhsT=actT[:, fc, il * 128:(il + 1) * 128],
                                                   rhs=W[:, fl, :], start=(fc == 0), stop=(fc == 63))
                            return ins
                        P.add("pe", mm, reads=[B_wsl[s]] + B_actT[16 * sd:16 * sd + 16], writes=PS[pbase:pbase + 4])
                    for il in range(4):
                        i = 4 * tcc + il
                        P.add("dve", lambda h, il=il, dc=dc, pbase=pbase: h.tensor_tensor(
                            out=x1t[il][:, dc * 512:(dc + 1) * 512], in0=ps[pbase + il][:, :],
                            in1=x1t[il][:, dc * 512:(dc + 1) * 512], op=ALU.add),
                            reads=[PS[pbase + il], B_x1t[il]], writes=[B_x1t[il]])
                        P.add("act", lambda h, il=il, dc=dc, i=i: h.activation(
                            out=junk6_ap, in_=x1t[il][:, dc * 512:(dc + 1) * 512], func=AF.Square,
                            accum_out=SM[:, c_ssq2 + 4 * i + dc:c_ssq2 + 4 * i + dc + 1]),
                            reads=[B_x1t[il]], writes=[B_junk6, B_sm])
                ssq2_v = SM[:, c_ssq2 + 16 * tcc:c_ssq2 + 16 * tcc + 16].rearrange("p (a b) -> p a b", b=4)
                P.add("dve", lambda h, tcc=tcc, ssq2_v=ssq2_v: h.tensor_reduce(
                    out=SM[:, c_t2 + 4 * tcc:c_t2 + 4 * tcc + 4], in_=ssq2_v, axis=AX.X, op=ALU.add),
                    reads=[B_sm], writes=[B_sm])
                rstd_ops(c_t2 + 4 * tcc, 4, c_s1 + 4 * tcc, c_s2 + 4 * tcc, 1.0 / D)
                for il in range(4):
                    i = 4 * tcc + il
                    P.add("dve", lambda h, i=i, il=il: h.scalar_tensor_tensor(
                        out=x1t[il], in0=x1t[il], scalar=SM[:, c_s2 + i:c_s2 + i + 1], in1=gfin, op0=ALU.mult,
                        op1=ALU.mult), reads=[B_x1t[il], B_sm, B_gf], writes=[B_x1t[il]])
                    P.add("sp", lambda h, i=i, il=il: h.dma_start(out=out_d[i * 128:(i + 1) * 128, :], in_=x1t[il]),
                          reads=[B_x1t[il]], writes=[Buf("out")], chan=ch_out)
        else:
            z = carve(0, [D], F32)
            Bz = Buf("z")
            P.add("pool", lambda h: h.memset(z, 0.0), writes=[Bz])
            P.add("sp", lambda h: h.dma_start(out=out_d[0:128, :], in_=z), reads=[Bz], writes=[Buf("out")], chan=ch_out)

        P.emit(block, engsem)
    return nc


_CACHE = {}


def _host_consts():
    if "c" not in _CACHE:
        _CACHE["c"] = (_onehot_tables(), (2.0 ** -(np.arange(NIT) + 1)).astype(np.float32))
    return _CACHE["c"]


def make_in_map(inputs, b):
    oh, pw2 = _host_consts()
    f = lambda a: np.ascontiguousarray(np.asarray(a, dtype=np.float32))
    return {
        "x": f(inputs["x"][b]),
        "w_in": f(inputs["w_in"][0]),
        "w_proj_a": f(inputs["w_proj_a"][0]),
        "w_proj_b": f(inputs["w_proj_b"][0]),
        "w_out": f(inputs["w_out"][0]),
        "w_mlp_up": f(inputs["w_mlp_up"][0]),
        "w_mlp_down": f(inputs["w_mlp_down"][0]),
        "norm_mix_g": f(inputs["norm_mix_g"][0]),
        "norm_mlp_g": f(inputs["norm_mlp_g"][0]),
        "norm_final_g": f(inputs["norm_final_g"]),
        "idx_k_norm_g": f(inputs["idx_k_norm_g"][0]),
        "idx_k_norm_b": f(inputs["idx_k_norm_b"][0]),
        "rel_bias_table": f(inputs["rel_bias_table"]),
        "onehot": oh,
        "pow2": pw2,
    }


def kernel(**inputs):
    nc = build_program()
    n = 8
    in_maps = [make_in_map(inputs, b) for b in range(n)]
    res = run_bass_kernel_spmd(nc, in_maps, core_ids=list(range(n)))
    return np.stack([np.asarray(r["out"], dtype=np.float32) for r in res.results], axis=0)
```
